# Optimizing a Trainium2 kernel written in Bass

```python
import jax
import jax.numpy as jnp
from jax import lax
import numpy as np

D_MODEL = 2048
BATCH = 4
SEQ = 4096
DEPTH = 1

EPS = 1e-6
GDN_HEADS = 8
GDN_DK = 128
GDN_DV = 128
CONV_WIDTH = 4
GDN_CHUNK = 64
RET_HEADS = 4
RET_DK = 256
RET_DV = 256
RET_CHUNK = 64
ROPE_BASE = 10000.0
N_EXPERTS = 32
TOP_K = 4
D_EXPERT = D_MODEL
SWIGLU_LIMIT = 7.0
SWIGLU_ALPHA = 1.702
MOE_BLOCK = 256

GDN_QK_W = GDN_HEADS * GDN_DK
GDN_V_W = GDN_HEADS * GDN_DV
RET_QK_W = RET_HEADS * RET_DK
RET_V_W = RET_HEADS * RET_DV
MIX_WIDTH = GDN_V_W + RET_V_W
GDN_CONV_CH = 2 * GDN_QK_W + GDN_V_W
IN_SIZES = (GDN_CONV_CH, GDN_V_W, GDN_HEADS, GDN_HEADS, RET_QK_W, RET_QK_W, RET_V_W, RET_V_W)
IN_COLS = GDN_CONV_CH + GDN_V_W + 2 * GDN_HEADS + 2 * RET_QK_W + 2 * RET_V_W

kernel_name = "hybrid_gdn_retention_moe_adaln_block"


def rmsnorm(x, g):
    x32 = x.astype(jnp.float32)
    y = x32 * lax.rsqrt(jnp.mean(x32 * x32, axis=-1, keepdims=True) + EPS)
    return (y * g.astype(jnp.float32)).astype(x.dtype)


def l2norm(x):
    return x * lax.rsqrt(jnp.sum(x * x, axis=-1, keepdims=True) + EPS)


def causal_depthwise_conv(u, w):
    return lax.conv_general_dilated(u, w[:, None, :].astype(u.dtype), window_strides=(1,),
                                    padding=[(w.shape[0] - 1, 0)],
                                    dimension_numbers=('NWC', 'WIO', 'NWC'),
                                    feature_group_count=u.shape[-1])


def apply_rotary(x, positions):
    half = x.shape[-1] // 2
    inv_freq = jnp.power(ROPE_BASE, -jnp.linspace(0.0, 1.0, half, dtype=jnp.float32))
    ang = positions.astype(jnp.float32)[..., None] * inv_freq
    cos = jnp.cos(ang)[:, :, None, :]
    sin = jnp.sin(ang)[:, :, None, :]
    x1, x2 = x[..., :half], x[..., half:]
    return jnp.concatenate([x1 * cos - x2 * sin, x2 * cos + x1 * sin], axis=-1)


def to_chunks(t, chunk):
    b, s, h = t.shape[:3]
    t = t.reshape((b, s // chunk, chunk, h) + t.shape[3:])
    perm = (0, 3, 1, 2) + tuple(range(4, t.ndim))
    return t.transpose(perm)


def from_chunks(t):
    n, b, h, c, d = t.shape
    return t.transpose(1, 0, 3, 2, 4).reshape(b, n * c, h, d)


def gated_delta_rule(q, k, v, beta, g_step):
    C = GDN_CHUNK
    dv = v.shape[-1]
    q, k, v = (to_chunks(t, C) for t in (q, k, v))
    beta = to_chunks(beta, C)
    g = jnp.cumsum(to_chunks(g_step, C), axis=-1)
    incl = jnp.tril(jnp.ones((C, C), dtype=bool))
    strict = jnp.tril(jnp.ones((C, C), dtype=bool), k=-1)
    diff = g[..., :, None] - g[..., None, :]
    decay_incl = jnp.exp(jnp.where(incl, diff, -jnp.inf))
    decay_strict = jnp.where(strict, decay_incl, 0.0)
    lower = beta[..., :, None] * jnp.einsum('bhnck,bhnmk->bhncm', k, k) * decay_strict
    a_mat = lower + jnp.eye(C, dtype=lower.dtype)
    rhs = jnp.concatenate([beta[..., None] * v, (beta * jnp.exp(g))[..., None] * k], axis=-1)
    sol = lax.linalg.triangular_solve(a_mat, rhs, left_side=True, lower=True, unit_diagonal=True)
    u, w = sol[..., :dv], sol[..., dv:]
    a_qk = jnp.einsum('bhnck,bhnmk->bhncm', q, k) * decay_incl
    q_dec = q * jnp.exp(g)[..., None]
    k_end = k * jnp.exp(g[..., -1:] - g)[..., None]
    chunk_decay = jnp.exp(g[..., -1])
    xs = tuple(jnp.moveaxis(t, 2, 0) for t in (q_dec, k_end, u, w, a_qk, chunk_decay))

    def step(state, inp):
        qd, ke, uc, wc, aqk, cd = inp
        delta = uc - jnp.einsum('bhck,bhkv->bhcv', wc, state)
        out = jnp.einsum('bhck,bhkv->bhcv', qd, state) + jnp.einsum('bhcm,bhmv->bhcv', aqk, delta)
        state = cd[..., None, None] * state + jnp.einsum('bhck,bhcv->bhkv', ke, delta)
        return state, out

    b, h, _, _, dk = q.shape
    s0 = jnp.zeros((b, h, dk, dv), jnp.float32)
    _, out = lax.scan(step, s0, xs)
    return from_chunks(out)


def multiscale_retention(q, k, v):
    C = RET_CHUNK
    h = q.shape[2]
    log_gamma = jnp.log1p(-jnp.exp2(-5.0 - jnp.arange(h, dtype=jnp.float32)))
    q, k, v = (to_chunks(t, C) for t in (q, k, v))
    pos = jnp.arange(C, dtype=jnp.float32)
    incl = jnp.tril(jnp.ones((C, C), dtype=bool))
    d_mat = jnp.exp(jnp.where(incl, (pos[:, None] - pos[None, :]) * log_gamma[:, None, None], -jnp.inf))
    scores = jnp.einsum('bhnck,bhnmk->bhncm', q, k) * d_mat[None, :, None]
    inner = jnp.einsum('bhncm,bhnmv->bhncv', scores, v)
    q_dec = q * jnp.exp((pos + 1.0) * log_gamma[:, None])[None, :, None, :, None]
    k_dec = k * jnp.exp((C - 1.0 - pos) * log_gamma[:, None])[None, :, None, :, None]
    chunk_decay = jnp.exp(C * log_gamma)[None, :, None, None]
    xs = tuple(jnp.moveaxis(t, 2, 0) for t in (q_dec, k_dec, v, inner))

    def step(state, inp):
        qd, kd, vc, inn = inp
        out = inn + jnp.einsum('bhck,bhkv->bhcv', qd, state)
        state = chunk_decay * state + jnp.einsum('bhck,bhcv->bhkv', kd, vc)
        return state, out

    b, _, _, _, dk = q.shape
    s0 = jnp.zeros((b, h, dk, v.shape[-1]), jnp.float32)
    _, out = lax.scan(step, s0, xs)
    return from_chunks(out)


def hybrid_mixer(h, positions, w_in, conv_w, a_log, dt_bias, gdn_norm_g, ret_norm_g, ret_norm_b, w_out):
    b, s, _ = h.shape
    f32 = jnp.float32
    proj = h @ w_in
    cuts = np.cumsum(IN_SIZES)[:-1].tolist()
    g_qkv, g_z, g_b, g_a, r_q, r_k, r_v, r_g = jnp.split(proj, cuts, axis=-1)
    g_qkv = jax.nn.silu(causal_depthwise_conv(g_qkv, conv_w)).astype(f32)
    gq, gk, gv = jnp.split(g_qkv, [GDN_QK_W, 2 * GDN_QK_W], axis=-1)
    gq = l2norm(gq.reshape(b, s, GDN_HEADS, GDN_DK)) * (GDN_DK ** -0.5)
    gk = l2norm(gk.reshape(b, s, GDN_HEADS, GDN_DK))
    gv = gv.reshape(b, s, GDN_HEADS, GDN_DV)
    beta = jax.nn.sigmoid(g_b.astype(f32))
    g_step = -jnp.exp(a_log.astype(f32)) * jax.nn.softplus(g_a.astype(f32) + dt_bias.astype(f32))
    o_g = gated_delta_rule(gq, gk, gv, beta, g_step)
    o_g = o_g * lax.rsqrt(jnp.mean(o_g * o_g, axis=-1, keepdims=True) + EPS) * gdn_norm_g.astype(f32)
    o_g = (o_g * jax.nn.silu(g_z.astype(f32).reshape(b, s, GDN_HEADS, GDN_DV))).reshape(b, s, GDN_V_W)
    rq = apply_rotary(r_q.astype(f32).reshape(b, s, RET_HEADS, RET_DK), positions)
    rk = apply_rotary(r_k.astype(f32).reshape(b, s, RET_HEADS, RET_DK), positions) * (RET_DK ** -0.5)
    rv = r_v.astype(f32).reshape(b, s, RET_HEADS, RET_DV)
    o_r = multiscale_retention(rq, rk, rv)
    mu = jnp.mean(o_r, axis=-1, keepdims=True)
    var = jnp.mean(jnp.square(o_r - mu), axis=-1, keepdims=True)
    o_r = ((o_r - mu) * lax.rsqrt(var + EPS)).reshape(b, s, RET_V_W)
    o_r = (o_r * ret_norm_g.astype(f32) + ret_norm_b.astype(f32)) * jax.nn.silu(r_g.astype(f32))
    mixed = jnp.concatenate([o_g, o_r], axis=-1).astype(h.dtype)
    return mixed @ w_out


def moe_ffn(h, w_router, b_router, w1, b1, w2, b2):
    b, s, d = h.shape
    t = b * s
    xf = h.reshape(t, d)
    logits = (xf @ w_router + b_router).astype(jnp.float32)
    top_val, top_idx = lax.top_k(logits, TOP_K)
    gates = jax.nn.softmax(top_val, axis=-1)
    flat_e = top_idx.reshape(-1)
    flat_t = jnp.repeat(jnp.arange(t, dtype=jnp.int32), TOP_K)
    flat_g = gates.reshape(-1)
    order = jnp.argsort(flat_e)
    se, st, sg = flat_e[order], flat_t[order], flat_g[order]
    counts = jnp.bincount(flat_e, length=N_EXPERTS)
    starts = jnp.cumsum(counts) - counts
    padded = (counts + MOE_BLOCK - 1) // MOE_BLOCK * MOE_BLOCK
    pad_ends = jnp.cumsum(padded)
    pad_starts = pad_ends - padded
    dest = pad_starts[se] + jnp.arange(t * TOP_K, dtype=jnp.int32) - starts[se]
    n_blocks = (t * TOP_K) // MOE_BLOCK + N_EXPERTS
    n_rows = n_blocks * MOE_BLOCK
    row_tok = jnp.full((n_rows,), t, jnp.int32).at[dest].set(st)
    row_gate = jnp.zeros((n_rows,), jnp.float32).at[dest].set(sg)
    block_e = jnp.minimum(jnp.searchsorted(pad_ends, jnp.arange(n_blocks) * MOE_BLOCK, side='right'),
                          N_EXPERTS - 1)
    x_pad = jnp.concatenate([xf, jnp.zeros((1, d), xf.dtype)], axis=0)

    def expert_block(args):
        tok, gate, e = args
        hid = x_pad[tok] @ w1[e] + b1[e]
        x_glu, x_lin = jnp.split(hid, 2, axis=-1)
        x_glu = jnp.minimum(x_glu, SWIGLU_LIMIT)
        x_lin = jnp.clip(x_lin, -SWIGLU_LIMIT, SWIGLU_LIMIT)
        act = x_glu * jax.nn.sigmoid(SWIGLU_ALPHA * x_glu) * (x_lin + 1.0)
        return (act @ w2[e] + b2[e]) * gate[:, None].astype(hid.dtype)

    ys = lax.map(expert_block, (row_tok.reshape(n_blocks, MOE_BLOCK),
                                row_gate.reshape(n_blocks, MOE_BLOCK), block_e))
    out = jax.ops.segment_sum(ys.reshape(n_rows, d), row_tok, num_segments=t + 1)[:t]
    return out.reshape(b, s, d)


def setup_inputs(seed: int = 0) -> dict:
    key = jax.random.key(seed)
    ks = jax.random.split(key, 24)
    f32 = jnp.float32
    L, D, E, F = DEPTH, D_MODEL, N_EXPERTS, D_EXPERT

    def nrm(k, shape, scale):
        return scale * jax.random.normal(k, shape, f32)

    x = jax.random.normal(ks[0], (BATCH, SEQ, D), f32)
    c = jax.random.normal(ks[1], (BATCH, D), f32)
    start = jax.random.randint(ks[2], (BATCH, 1), 0, 1024, dtype=jnp.int32)
    positions = start + jnp.arange(SEQ, dtype=jnp.int32)[None, :]
    return {
        'x': x,
        'c': c,
        'positions': positions,
        'ada_w': nrm(ks[3], (L, D, 6 * D), D ** -0.5),
        'ada_b': nrm(ks[4], (L, 6 * D), 0.02),
        'norm1_g': 1.0 + nrm(ks[5], (L, D), 0.02),
        'w_in': nrm(ks[6], (L, D, IN_COLS), D ** -0.5),
        'conv_w': nrm(ks[7], (L, CONV_WIDTH, GDN_CONV_CH), CONV_WIDTH ** -0.5),
        'gdn_a_log': jnp.log(jax.random.uniform(ks[8], (L, GDN_HEADS), f32, 1.0, 16.0)),
        'gdn_dt_bias': nrm(ks[9], (L, GDN_HEADS), 0.5),
        'gdn_norm_g': 1.0 + nrm(ks[10], (L, GDN_DV), 0.02),
        'ret_norm_g': 1.0 + nrm(ks[11], (L, RET_V_W), 0.02),
        'ret_norm_b': nrm(ks[12], (L, RET_V_W), 0.02),
        'w_out': nrm(ks[13], (L, MIX_WIDTH, D), MIX_WIDTH ** -0.5),
        'norm2_g': 1.0 + nrm(ks[14], (L, D), 0.02),
        'w_router': nrm(ks[15], (L, D, E), D ** -0.5),
        'b_router': nrm(ks[16], (L, E), 0.01),
        'w1': nrm(ks[17], (L, E, D, 2 * F), D ** -0.5),
        'b1': nrm(ks[18], (L, E, 2 * F), 0.01),
        'w2': nrm(ks[19], (L, E, F, D), F ** -0.5),
        'b2': nrm(ks[20], (L, E, D), 0.01),
        'final_norm_g': 1.0 + nrm(ks[21], (D,), 0.02),
    }


def reference(x, c, positions, ada_w, ada_b, norm1_g, w_in, conv_w, gdn_a_log, gdn_dt_bias, gdn_norm_g,
              ret_norm_g, ret_norm_b, w_out, norm2_g, w_router, b_router, w1, b1, w2, b2, final_norm_g):
    cond = jax.nn.silu(c)
    for l in range(DEPTH):
        mod = (cond @ ada_w[l] + ada_b[l])[:, None, :]
        sh1, sc1, gt1, sh2, sc2, gt2 = jnp.split(mod, 6, axis=-1)
        h = rmsnorm(x, norm1_g[l]) * (1.0 + sc1) + sh1
        x = x + gt1 * hybrid_mixer(h, positions, w_in[l], conv_w[l], gdn_a_log[l], gdn_dt_bias[l],
                                   gdn_norm_g[l], ret_norm_g[l], ret_norm_b[l], w_out[l])
        h = rmsnorm(x, norm2_g[l]) * (1.0 + sc2) + sh2
        x = x + gt2 * moe_ffn(h, w_router[l], b_router[l], w1[l], b1[l], w2[l], b2[l])
    return rmsnorm(x, final_norm_g)
```

```python
import contextlib
import numpy as np
import concourse.bass as bass
import concourse.mybir as mybir
from concourse.bass_utils import run_bass_kernel_spmd

F32 = mybir.dt.float32
BF16 = mybir.dt.bfloat16
I32 = mybir.dt.int32
AF = mybir.ActivationFunctionType
OP = mybir.AluOpType

D = 2048
NTOK = 2048
TB = 256
NBLK = 8
TG = 512
NTG = 4
NCH = TB // 128
E = 32
EPS = 1e-6
IN_COLS = 8208
SEM_LIM = 20000
MATH_PI = float(np.pi)


class View:
    __slots__ = ("t", "ap")

    def __init__(self, t, ap):
        self.t = t
        self.ap = ap


class T:
    def __init__(self, h, excl=False):
        self.h = h
        self.lastw = None
        self.reads = {}
        self.excl = excl

    def __getitem__(self, idx):
        return View(self, self.h[idx])


class KB:
    def __init__(self, nc):
        self.nc = nc
        self.es = contextlib.ExitStack()
        self.eng = {"pe": nc.tensor, "act": nc.scalar, "dve": nc.vector, "pool": nc.gpsimd, "sp": nc.sync}
        self.cur = {}
        self.seen = {k: {} for k in self.eng}
        self.nsem = 0
        self.last_ev = {}
        self.dma_pool = {}
        self.dma_idx = {}
        self.all_dma = {}
        self.dead = False
        for k in ("pe", "act", "dve", "pool"):
            self._new_epoch(k)
        for q in ("sp", "pool"):
            self.dma_pool[q] = [[self._sem("dq_%s%d" % (q, i)), 0] for i in range(12)]
            self.dma_idx[q] = 0

    def _sem(self, name):
        self.nsem += 1
        h = self.es.enter_context(self.nc.semaphore("%s_%d" % (name, self.nsem)))
        return (self.nsem, h)

    def _new_epoch(self, e):
        self.cur[e] = [self._sem("e_" + e), 0]

    def sb(self, name, shape, dt, stack=None):
        return T((stack or self.es).enter_context(self.nc.sbuf_tensor("s_" + name, shape, dt)))

    def _wait(self, e, need):
        seen = self.seen[e]
        own = self.cur[e][0][0] if e in self.cur else None
        for (key, h, v) in need:
            if e == "pe" and key == own:
                continue
            if seen.get(key, 0) >= v:
                continue
            self.eng[e].wait_ge(h, v)
            seen[key] = v

    def _deps(self, R, W):
        need = []
        for t in R:
            if t is not None and t.lastw is not None:
                need.append(t.lastw)
            if t is not None and t.excl:
                need.extend(t.reads.values())
        for t in W:
            if t is None:
                continue
            if t.lastw is not None:
                need.append(t.lastw)
            need.extend(t.reads.values())
        return need

    def _record(self, ev, R, W):
        for t in R:
            if t is not None:
                t.reads[ev[0]] = ev
        for t in W:
            if t is not None:
                t.lastw = ev
                t.reads = {}

    def op(self, e, fn, R, W):
        if self.dead:
            return
        self._wait(e, self._deps(R, W))
        inst = fn()
        c = self.cur[e]
        c[1] += 1
        inst.then_inc(c[0][1], 1)
        ev = (c[0][0], c[0][1], c[1])
        self.last_ev[c[0][0]] = (e, ev)
        if c[1] >= 30000:
            self._new_epoch(e)
        self._record(ev, R, W)

    def dma(self, q, out, in_, **kw):
        if self.dead:
            return None
        R = [in_.t]
        W = [out.t]
        pool = self.dma_pool[q]
        i = self.dma_idx[q]
        self.dma_idx[q] = (i + 1) % len(pool)
        slot = pool[i]
        need = self._deps(R, W)
        if slot[1] > 0:
            need.append((slot[0][0], slot[0][1], slot[1]))
        self._wait(q, need)
        inst = self.eng[q].dma_start(out=out.ap, in_=in_.ap, **kw)
        slot[1] += 16
        if slot[1] >= SEM_LIM:
            pool[i] = [self._sem("dq_%s" % q), 0]
            slot2 = pool[i]
            slot2[1] = 16
            inst.then_inc(slot2[0][1], 16)
            ev = (slot2[0][0], slot2[0][1], 16)
        else:
            inst.then_inc(slot[0][1], 16)
            ev = (slot[0][0], slot[0][1], slot[1])
        self.all_dma[ev[0]] = ev
        self._record(ev, R, W)
        return ev

    def maybe_roll(self, lim=8000):
        if self.dead:
            return
        for e in list(self.cur):
            if self.cur[e][1] >= lim:
                self._new_epoch(e)

    def barrier(self):
        if self.dead:
            return
        evs = [ev for (_, ev) in self.last_ev.values()] + list(self.all_dma.values())
        for e in self.eng:
            self._wait_all(e, evs)

    def _wait_all(self, e, evs):
        seen = self.seen[e]
        for (key, h, v) in evs:
            if seen.get(key, 0) >= v:
                continue
            self.eng[e].wait_ge(h, v)
            seen[key] = v

    def mm(self, out, lhsT, rhs, start=True, stop=True):
        self.op("pe", lambda: self.nc.tensor.matmul(out.ap, lhsT=lhsT.ap, rhs=rhs.ap, start=start, stop=stop),
                [lhsT.t, rhs.t], [out.t])

    def tr(self, out, in_, ident):
        self.op("pe", lambda: self.nc.tensor.transpose(out.ap, in_.ap, ident.ap), [in_.t, ident.t], [out.t])

    def act(self, out, in_, func, bias=None, scale=None, accum=None):
        R = [in_.t]
        W = [out.t]
        kw = {}
        if bias is not None:
            if isinstance(bias, View):
                R.append(bias.t)
                kw["bias"] = bias.ap
            else:
                kw["bias"] = float(bias)
        if scale is not None:
            if isinstance(scale, View):
                R.append(scale.t)
                kw["scale"] = scale.ap
            else:
                kw["scale"] = float(scale)
        if accum is not None:
            W.append(accum.t)
            kw["accum_out"] = accum.ap
        self.op("act", lambda: self.nc.scalar.activation(out=out.ap, in_=in_.ap, func=func, **kw), R, W)

    def ts(self, out, in0, s1, op0, s2=None, op1=None, eng="dve"):
        R = [in0.t]

        def cv(s):
            if isinstance(s, View):
                R.append(s.t)
                return s.ap
            return None if s is None else float(s)
        a1 = cv(s1)
        a2 = cv(s2)
        kw = {}
        if op1 is not None:
            kw["op1"] = op1
        E_ = self.eng[eng]
        self.op(eng, lambda: E_.tensor_scalar(out=out.ap, in0=in0.ap, scalar1=a1, scalar2=a2, op0=op0, **kw),
                R, [out.t])

    def tt(self, out, in0, in1, op, eng="dve"):
        E_ = self.eng[eng]
        self.op(eng, lambda: E_.tensor_tensor(out=out.ap, in0=in0.ap, in1=in1.ap, op=op),
                [in0.t, in1.t], [out.t])

    def stt(self, out, in0, scalar, in1, op0, op1):
        R = [in0.t, in1.t]
        if isinstance(scalar, View):
            R.append(scalar.t)
            s = scalar.ap
        else:
            s = float(scalar)
        self.op("dve", lambda: self.nc.vector.scalar_tensor_tensor(out=out.ap, in0=in0.ap, scalar=s, in1=in1.ap,
                                                                   op0=op0, op1=op1), R, [out.t])

    def copy(self, out, in_, eng="dve"):
        if eng == "act":
            self.act(out, in_, AF.Identity)
        else:
            E_ = self.eng[eng]
            self.op(eng, lambda: E_.tensor_copy(out=out.ap, in_=in_.ap), [in_.t], [out.t])

    def memset(self, out, val, eng="dve"):
        E_ = self.eng[eng]
        self.op(eng, lambda: E_.memset(out.ap, val), [], [out.t])

    def recip(self, out, in_):
        self.op("dve", lambda: self.nc.vector.reciprocal(out=out.ap, in_=in_.ap), [in_.t], [out.t])


class _Stop(Exception):
    pass


def build_program(n_exp=E, dbg=False, do_moe=True, stop=None):
    nc = bass.Bass("TRN2", target_bir_lowering=False)
    kb = KB(nc)

    def dram_in(name, shape, dt=F32):
        return nc.dram_tensor(name, shape, dt, kind="ExternalInput").ap()

    xm = dram_in("xm", [NTOK, D])
    xp = dram_in("xp", [NTOK, D])
    posm = dram_in("posm", [128, NTOK], I32)
    posp = dram_in("posp", [128, NTOK], I32)
    flag_d = dram_in("flag", [128, 1])
    c_d = dram_in("c", [128, 16])
    ada_w = dram_in("ada_w", [D, 6 * D])
    ada_b_d = dram_in("ada_b", [128, 6 * D])
    n1g_d = dram_in("n1g", [128, D])
    n2g_d = dram_in("n2g", [128, D])
    fng_d = dram_in("fng", [128, D])
    w_in = dram_in("w_in", [D, IN_COLS])
    convw_d = dram_in("convw", [128, 24 * 4])
    alog_d = dram_in("alog", [128, 8])
    dtb_d = dram_in("dtb", [128, 8])
    gng_d = dram_in("gng", [128, 128])
    rng_d = dram_in("rng", [128, 1024])
    rnb_d = dram_in("rnb", [128, 1024])
    w_out = dram_in("w_out", [D, D])
    wr_d = dram_in("wr", [128, 16 * 32])
    br_d = dram_in("br", [128, 32])
    w1 = dram_in("w1", [n_exp, D, 2 * D])
    b1_d = dram_in("b1", [128, E * 32])
    w2 = dram_in("w2", [n_exp, D, D])
    b2_d = dram_in("b2", [E, D])
    consts_d = dram_in("consts", [128, 1536])
    out_d = nc.dram_tensor("out", [NTOK, D], F32, kind="ExternalOutput").ap()
    ik = "ExternalOutput" if dbg else "Internal"
    x2s_ap = nc.dram_tensor("x2s", [NTOK, D], F32, kind=ik).ap()
    h2s_ap = nc.dram_tensor("h2s", [128, 16 * NTOK], BF16, kind=ik).ap()
    gts_ap = nc.dram_tensor("gts", [NTOK, E], F32, kind=ik).ap()
    x2s = [T(None) for _ in range(NTG)]
    h2s = [T(None) for _ in range(NTG)]
    gts = [T(None) for _ in range(NTG)]

    def DV(ap):
        return View(None, ap)

    def r3(v, j):
        return View(v.t, v.ap.rearrange("p (j t) -> p j t", j=j))

    def CHK(tag):
        if stop == tag and not kb.dead:
            kb.barrier()
            kb.dead = True

    with kb.es:
      try:
        cst = kb.sb("cst", [128, 1536], F32)
        ident = cst[:, 0:128]
        TRI = cst[:, 128:256]
        MLS = cst[:, 256:384]
        MLI = cst[:, 384:512]
        ONES = cst[:, 512:640]

        def RDT(h):
            return cst[:, 640 + h * 128: 640 + (h + 1) * 128]

        def RQD(h):
            return cst[:, 1152 + h: 1153 + h]

        def RKD(h):
            return cst[:, 1156 + h: 1157 + h]
        INVF = cst[:, 1160:1161]
        kb.dma("sp", cst[:, :], DV(consts_d[:, :]))
        flag = kb.sb("flag", [128, 1], F32)
        kb.dma("sp", flag[:, :], DV(flag_d[:, :]))
        gt1row = kb.sb("gt1row", [128, D], F32)
        gt2row = kb.sb("gt2row", [128, D], F32)
        modc = kb.sb("modc", [128, 64], F32)
        ps = [T(kb.es.enter_context(nc.psum_tensor("ps%d" % i, [128, 512], F32)), excl=True) for i in range(8)]
        psi = [0]

        def PS():
            psi[0] = (psi[0] + 1) % 8
            return ps[psi[0]]

        def load_w(src2d, c0, ncols, dst):
            kb.dma("pool", dst[:, :, 0:ncols], DV(src2d[:, c0:c0 + ncols].rearrange("(k p) c -> p k c", p=128)))
            return dst

        with contextlib.ExitStack() as st0:
            cond = kb.sb("cond", [128, 16], F32, st0)
            kb.dma("sp", cond[:, :], DV(c_d[:, :]))
            kb.act(cond[:, :], cond[:, :], AF.Silu)
            condb = kb.sb("condb", [128, 16, 128], F32, st0)
            for k in range(16):
                kb.ts(condb[:, k, :], ONES, cond[:, k:k + 1], OP.mult)
            adab = kb.sb("adab", [128, 6 * D], F32, st0)
            kb.dma("sp", adab[:, :], DV(ada_b_d[:, :]))
            aw = [kb.sb("aw%d" % i, [128, 16, 512], F32, st0) for i in range(2)]
            modr = kb.sb("modr", [128, 6, D], F32, st0)
            for g in range(24):
                a = aw[g % 2]
                kb.dma("sp", a[:, :, :], DV(ada_w[:, g * 512:(g + 1) * 512].rearrange("(k p) c -> p k c", p=128)))
                p = PS()
                for k in range(16):
                    kb.mm(p[:, :], condb[:, k, :], a[:, k, :], start=(k == 0), stop=(k == 15))
                kb.tt(modr[:, g // 4, (g % 4) * 512:(g % 4 + 1) * 512], p[:, :], adab[:, g * 512:(g + 1) * 512], OP.add)
            tmpg = kb.sb("tmpg", [128, D], F32, st0)
            for (src, gd) in ((1, n1g_d), (4, n2g_d)):
                kb.dma("sp", tmpg[:, :], DV(gd[:, :]))
                kb.stt(modr[:, src, :], modr[:, src, :], 1.0, tmpg[:, :], OP.add, OP.mult)
            kb.copy(gt1row[:, :], modr[:, 2, :])
            kb.copy(gt2row[:, :], modr[:, 5, :], eng="pool")
            for vi, src in enumerate((1, 0, 4, 3)):
                for g in range(4):
                    p = PS()
                    for j in range(4):
                        kc = g * 4 + j
                        kb.tr(p[:, j * 128:(j + 1) * 128], modr[:, src, kc * 128:(kc + 1) * 128], ident)
                    for j in range(4):
                        kc = g * 4 + j
                        kb.copy(modc[:, vi * 16 + kc: vi * 16 + kc + 1], p[:, j * 128: j * 128 + 1])
            kb.barrier()
        CHK('p0')

        with contextlib.ExitStack() as st1:
            S_g = kb.sb("S_g", [128, 8, 128], F32, st1)
            S_r = kb.sb("S_r", [128, 8, 256], F32, st1)
            carry = kb.sb("carry", [128, 24, 4], F32, st1)
            kb.memset(S_g[:, :, :], 0.0)
            kb.memset(S_r[:, :, :], 0.0)
            kb.memset(carry[:, :, :], 0.0)
            convw = kb.sb("convw", [128, 24, 4], F32, st1)
            kb.dma("sp", convw[:, :, :], DV(convw_d[:, :].rearrange("p (c i) -> p c i", i=4)))
            negA = kb.sb("negA", [128, 8], F32, st1)
            dtb = kb.sb("dtb", [128, 8], F32, st1)
            kb.dma("sp", negA[:, :], DV(alog_d[:, :]))
            kb.dma("sp", dtb[:, :], DV(dtb_d[:, :]))
            kb.act(negA[:, :], negA[:, :], AF.Exp)
            kb.ts(negA[:, :], negA[:, :], -1.0, OP.mult)
            gng = kb.sb("gng", [128, 128], F32, st1)
            kb.dma("sp", gng[:, :], DV(gng_d[:, :]))
            rng = kb.sb("rng", [128, 1024], F32, st1)
            rnb = kb.sb("rnb", [128, 1024], F32, st1)
            kb.dma("sp", rng[:, :], DV(rng_d[:, :]))
            kb.dma("sp", rnb[:, :], DV(rnb_d[:, :]))
            wr = kb.sb("wr", [128, 16, 32], F32, st1)
            kb.dma("sp", wr[:, :, :], DV(wr_d[:, :].rearrange("p (k e) -> p k e", e=32)))
            br = kb.sb("br", [128, 32], F32, st1)
            kb.dma("sp", br[:, :], DV(br_d[:, :]))

            xt = [kb.sb("xt%d" % i, [128, D], F32, st1) for i in range(2)]
            hT = kb.sb("hT", [128, 16, TB], BF16, st1)
            mixedT = kb.sb("mixedT", [128, 16, TB], BF16, st1)
            h2f = kb.sb("h2f", [128, 16, 128], F32, st1)
            small = kb.sb("small", [128, 64], F32, st1)
            ba = kb.sb("ba", [128, NCH, 16], F32, st1)
            beta = kb.sb("beta", [128, NCH, 8], F32, st1)
            gs_ = kb.sb("gs", [128, NCH, 8], F32, st1)
            gcol = kb.sb("gcol", [128, NCH, 8], F32, st1)
            eg = kb.sb("eg", [128, NCH, 8], F32, st1)
            nbeg = kb.sb("nbeg", [128, NCH, 8], F32, st1)
            kes = kb.sb("kes", [128, NCH, 8], F32, st1)
            cdt = kb.sb("cd", [128, NCH, 8], F32, st1)
            tmp8 = kb.sb("tmp8", [128, NCH, 8], F32, st1)
            tmp8b = kb.sb("tmp8b", [128, NCH, 8], F32, st1)
            cosT = kb.sb("cosT", [128, TB], F32, st1)
            sinT = kb.sb("sinT", [128, TB], F32, st1)
            posi = kb.sb("posi", [128, TB], I32, st1)
            uw = [kb.sb("uw%d" % i, [128, TB + 3], F32, st1) for i in range(2)]
            NFM = 12
            fm = [kb.sb("fm%d" % i, [128, TB], F32, st1) for i in range(NFM)]
            fmi = [0]

            def FM():
                fmi[0] = (fmi[0] + 1) % NFM
                return fm[fmi[0]]
            slot_q = [kb.sb("slq%d" % i, [128, TB], F32, st1) for i in range(4)]
            slot_k = [kb.sb("slk%d" % i, [128, TB], F32, st1) for i in range(4)]
            slot_kt = [kb.sb("slkt%d" % i, [128, NCH, 128], F32, st1) for i in range(4)]
            slot_vt = [kb.sb("slvt%d" % i, [128, NCH, 128], F32, st1) for i in range(4)]
            slot_z = [kb.sb("slz%d" % i, [128, NCH, 128], F32, st1) for i in range(4)]
            slot_aq = [kb.sb("slaq%d" % i, [128, 128], F32, st1) for i in range(4)]
            slot_sq = [[kb.sb("slsq%d_%d" % (i, j), [128, 128], F32, st1) for j in range(10)] for i in range(4)]
            pre_v = kb.sb("pre_v", [128, TB], F32, st1)
            pre_s2 = kb.sb("pre_s2", [128, TB], F32, st1)
            NSQ = 4
            sq = [kb.sb("sq%d" % i, [128, 128], F32, st1) for i in range(NSQ)]
            sqi = [0]

            def SQ():
                sqi[0] = (sqi[0] + 1) % NSQ
                return sq[sqi[0]]
            tk = [kb.sb("tk%d" % i, [128, NCH, 256], F32, st1) for i in range(4)]
            wq = [kb.sb("wq%d" % i, [128, 16, 128], BF16, st1) for i in range(3)]
            wqi = [0]

            def WQ():
                wqi[0] = (wqi[0] + 1) % 3
                return wq[wqi[0]]
            wb1 = [kb.sb("wb1_%d" % i, [128, 16, 256], BF16, st1) for i in range(2)]
            wb1i = [0]

            def WB1():
                wb1i[0] = (wb1i[0] + 1) % 2
                return wb1[wb1i[0]]
            col = [kb.sb("col%d" % i, [128, 8], F32, st1) for i in range(8)]
            coli = [0]

            def COL():
                coli[0] = (coli[0] + 1) % 8
                return col[coli[0]]
            o256 = [kb.sb("o256_%d" % i, [128, 256], F32, st1) for i in range(4)]
            o2i = [0]

            def O256():
                o2i[0] = (o2i[0] + 1) % 4
                return o256[o2i[0]]

            def rstd_from_ss(ss, n, dst):
                kb.ts(dst, ss, 1.0 / n, OP.mult, EPS, OP.add)
                kb.act(dst, dst, AF.Sqrt)
                kb.recip(dst, dst)

            def norm_mod_T(xtile, gbase, sbase, dstT, tt, f32T=None):
                c_ = COL()
                j_ = FM()
                for q4 in range(D // TB):
                    kb.act(j_[:, :], xtile[:, q4 * TB:(q4 + 1) * TB], AF.Square, accum=c_[:, 2 + (q4 % 4):3 + (q4 % 4)]) \
                        if False else None
                kb.act(xtile_junk[:, :], xtile[:, :], AF.Square, accum=c_[:, 0:1])
                rstd_from_ss(c_[:, 0:1], float(D), c_[:, 1:2])
                kb.ts(xtile[:, :], xtile[:, :], c_[:, 1:2], OP.mult)
                for g in range(4):
                    p = PS()
                    for j in range(4):
                        kc = g * 4 + j
                        kb.tr(p[:, j * 128:(j + 1) * 128], xtile[:, kc * 128:(kc + 1) * 128], ident)
                    for j in range(4):
                        kc = g * 4 + j
                        kb.act(dstT[:, kc, tt * 128:(tt + 1) * 128], p[:, j * 128:(j + 1) * 128], AF.Identity,
                               scale=modc[:, gbase + kc: gbase + kc + 1], bias=modc[:, sbase + kc: sbase + kc + 1])
                        if f32T is not None:
                            kb.ts(f32T[:, kc, :], p[:, j * 128:(j + 1) * 128], modc[:, gbase + kc: gbase + kc + 1], OP.mult,
                                  modc[:, sbase + kc: sbase + kc + 1], OP.add)

            xtile_junk = kb.sb("xjunk", [128, D], BF16, st1)

            def proj_fm(wtile, dst_ps):
                for k in range(16):
                    kb.mm(dst_ps, wtile[:, k, 0:128], hT[:, k, :], start=(k == 0), stop=(k == 15))

            def proj_tm(wtile, ncols, tt, dst_ps):
                for k in range(16):
                    kb.mm(dst_ps, hT[:, k, tt * 128:(tt + 1) * 128], wtile[:, k, 0:ncols], start=(k == 0), stop=(k == 15))

            for blk in range(2 * NBLK):
                kb.maybe_roll()
                main = blk >= NBLK
                xsrc = xm if main else xp
                psrc = posm if main else posp
                t0 = (blk % NBLK) * TB
                for tt in range(NCH):
                    x_ = xt[tt % 2]
                    kb.dma("sp", x_[:, :], DV(xsrc[t0 + tt * 128: t0 + (tt + 1) * 128, :]))
                    norm_mod_T(x_, 0, 16, hT, tt)
                CHK('norm1')
                ang, ang2 = FM(), FM()
                kb.dma("sp", posi[:, :], DV(psrc[:, t0:t0 + TB]))
                kb.copy(ang[:, :], posi[:, :])
                kb.ts(ang[:, :], ang[:, :], INVF, OP.mult)
                for (shift, dst) in ((0.0, sinT), (MATH_PI / 2, cosT)):
                    kb.ts(ang2[:, :], ang[:, :], shift, OP.add, 1.0 / (2 * MATH_PI), OP.mult)
                    kb.copy(posi[:, :], ang2[:, :])
                    kb.copy(ang2[:, :], posi[:, :])
                    kb.stt(ang2[:, :], ang2[:, :], -2 * MATH_PI, ang[:, :], OP.mult, OP.add)
                    kb.ts(ang2[:, :], ang2[:, :], shift, OP.add)
                    m_ = FM()
                    kb.ts(m_[:, :], ang2[:, :], MATH_PI, OP.is_gt)
                    kb.stt(ang2[:, :], m_[:, :], -2 * MATH_PI, ang2[:, :], OP.mult, OP.add)
                    kb.ts(m_[:, :], ang2[:, :], -MATH_PI, OP.is_lt)
                    kb.stt(ang2[:, :], m_[:, :], 2 * MATH_PI, ang2[:, :], OP.mult, OP.add)
                    kb.ts(ang2[:, :], ang2[:, :], MATH_PI, OP.min, -MATH_PI, OP.max)
                    kb.act(dst[:, :], ang2[:, :], AF.Sin)
                CHK('rot')
                wba = WQ()
                kb.dma("pool", wba[:, :, 0:16], DV(w_in[:, 4096:4112].rearrange("(k p) c -> p k c", p=128)))
                for tt in range(NCH):
                    p = PS()
                    proj_tm(wba, 16, tt, p[:, 0:16])
                    kb.copy(ba[:, tt, :], p[:, 0:16])
                kb.act(beta[:, :, :], ba[:, :, 0:8], AF.Sigmoid)
                for tt in range(NCH):
                    kb.tt(tmp8[:, tt, :], ba[:, tt, 8:16], dtb[:, :], OP.add)
                kb.stt(tmp8b[:, :, :], tmp8[:, :, :], -1.0, tmp8[:, :, :], OP.mult, OP.max)
                kb.act(tmp8b[:, :, :], tmp8b[:, :, :], AF.Exp, scale=-1.0)
                kb.act(tmp8b[:, :, :], tmp8b[:, :, :], AF.Ln, bias=1.0)
                kb.stt(tmp8[:, :, :], tmp8[:, :, :], 0.0, tmp8b[:, :, :], OP.max, OP.add)
                for tt in range(NCH):
                    kb.tt(gs_[:, tt, :], tmp8[:, tt, :], negA[:, :], OP.mult)
                for tt in range(NCH):
                    p = PS()
                    kb.mm(p[:, 0:8], TRI, gs_[:, tt, :])
                    kb.mm(p[:, 8:16], ONES, gs_[:, tt, :])
                    kb.copy(gcol[:, tt, :], p[:, 0:8])
                    kb.act(eg[:, tt, :], p[:, 0:8], AF.Exp)
                    kb.act(cdt[:, tt, :], p[:, 8:16], AF.Exp)
                    kb.tt(tmp8b[:, tt, :], p[:, 8:16], gcol[:, tt, :], OP.subtract)
                    kb.act(kes[:, tt, :], tmp8b[:, tt, :], AF.Exp)
                kb.stt(nbeg[:, :, :], beta[:, :, :], -1.0, eg[:, :, :], OP.mult, OP.mult)

                CHK('ba')
                def gdn_pre(h, sl):
                    qT, kT = slot_q[sl], slot_k[sl]
                    vT = None
                    for which in range(3):
                        cc = which * 8 + h
                        wt_ = load_w(w_in, cc * 128, 128, WQ())
                        p = PS()
                        proj_fm(wt_, p[:, 0:TB])
                        u = uw[which % 2]
                        kb.copy(u[:, 0:3], carry[:, cc, 0:3], eng="pool")
                        kb.act(u[:, 3:TB + 3], p[:, 0:TB], AF.Identity)
                        y = (qT, kT, pre_v)[which]
                        kb.ts(y[:, :], u[:, 0:TB], convw[:, cc, 0:1], OP.mult)
                        for i in range(1, 4):
                            kb.stt(y[:, :], u[:, i:TB + i], convw[:, cc, i:i + 1], y[:, :], OP.mult, OP.add)
                        kb.copy(carry[:, cc, 0:3], u[:, TB:TB + 3], eng="pool")
                        kb.act(y[:, :], y[:, :], AF.Silu)
                        if which == 2:
                            vT = y
                    for (src, extra) in ((qT, float(np.log(128.0 ** -0.5))), (kT, 0.0)):
                        s2 = pre_s2
                        kb.act(s2[:, :], src[:, :], AF.Square)
                        p = PS()
                        kb.mm(p[:, 0:TB], ONES, s2[:, :])
                        kb.act(s2[:, :], p[:, 0:TB], AF.Ln, bias=EPS)
                        kb.act(s2[:, :], s2[:, :], AF.Exp, scale=-0.5, bias=extra)
                        kb.tt(src[:, :], src[:, :], s2[:, :], OP.mult)
                    for (src, dst) in ((kT, slot_kt[sl]), (vT, slot_vt[sl])):
                        p = PS()
                        for c in range(NCH):
                            kb.tr(p[:, c * 128:(c + 1) * 128], src[:, c * 128:(c + 1) * 128], ident)
                        kb.act(dst[:, :, :], r3(p[:, 0:TB], NCH), AF.Identity)
                    if main:
                        wz = load_w(w_in, 3072 + h * 128, 128, WQ())
                        for tt in range(NCH):
                            p = PS()
                            proj_tm(wz, 128, tt, p[:, 0:128])
                            kb.act(slot_z[sl][:, tt, :], p[:, 0:128], AF.Silu)

                def gdn_chunks(h, sl):
                    qT, kT, ktok, vtok, zs = slot_q[sl], slot_k[sl], slot_kt[sl], slot_vt[sl], slot_z[sl]
                    pool_ = slot_sq[sl]
                    pi = [0]

                    def SQh():
                        pi[0] = (pi[0] + 1) % len(pool_)
                        return pool_[pi[0]]
                    AQKT = slot_aq[sl]
                    for c in range(NCH):
                        cs = slice(c * 128, (c + 1) * 128)
                        bcol = beta[:, c, h:h + 1]
                        GSB = SQh()
                        kb.ts(GSB[:, :], ONES, gs_[:, c, h:h + 1], OP.mult, eng="pool")
                        yield
                        pRB = PS()
                        kb.mm(pRB[:, 0:128], GSB[:, :], TRI)
                        NE = SQh()
                        kb.ts(NE[:, :], pRB[:, 0:128], gcol[:, c, h:h + 1], OP.subtract, 0.0, OP.max)
                        EX = SQh()
                        kb.act(EX[:, :], NE[:, :], AF.Exp, scale=-1.0)
                        yield
                        ML = SQh()
                        kb.stt(ML[:, :], EX[:, :], bcol, MLS, OP.mult, OP.mult)
                        EXI = SQh()
                        kb.tt(EXI[:, :], EX[:, :], MLI, OP.mult, eng="pool")
                        yield
                        pG = PS()
                        kb.mm(pG[:, 0:128], kT[:, cs], kT[:, cs])
                        kb.mm(pG[:, 128:256], qT[:, cs], kT[:, cs])
                        L = SQh()
                        kb.tt(L[:, :], pG[:, 0:128], ML[:, :], OP.mult)
                        AQK = SQh()
                        kb.tt(AQK[:, :], pG[:, 128:256], EXI[:, :], OP.mult)
                        yield
                        pT = PS()
                        kb.tr(pT[:, 0:128], L[:, :], ident)
                        kb.tr(pT[:, 128:256], AQK[:, :], ident)
                        U = SQh()
                        kb.copy(U[:, :], pT[:, 0:128], eng="act")
                        kb.copy(AQKT[:, :], pT[:, 128:256], eng="act")
                        yield
                        P = SQh()
                        kb.tt(P[:, :], ident, U[:, :], OP.subtract)
                        Lp, Up = L, U
                        for k in range(1, 7):
                            pp = PS()
                            kb.mm(pp[:, 0:128], Up[:, :], Lp[:, :])
                            if k < 6:
                                kb.mm(pp[:, 128:256], Lp[:, :], Up[:, :])
                            Ln = SQh()
                            kb.copy(Ln[:, :], pp[:, 0:128], eng="act")
                            if k < 6:
                                Un = SQh()
                                kb.copy(Un[:, :], pp[:, 128:256])
                            else:
                                Un = None
                            yield
                            pq = PS()
                            kb.mm(pq[:, 0:128], Ln[:, :], P[:, :])
                            Pn = SQh()
                            kb.tt(Pn[:, :], pq[:, 0:128], P[:, :], OP.add)
                            P, Lp, Up = Pn, Ln, Un
                            yield
                        BV = SQh()
                        kb.ts(BV[:, :], vtok[:, c, :], bcol, OP.mult, eng="pool")
                        KE = SQh()
                        kb.ts(KE[:, :], ktok[:, c, :], kes[:, c, h:h + 1], OP.mult, eng="pool")
                        yield
                        pK = PS()
                        kb.mm(pK[:, 0:128], kT[:, cs], S_g[:, h, :])
                        Rr = SQh()
                        kb.stt(Rr[:, :], pK[:, 0:128], nbeg[:, c, h:h + 1], BV[:, :], OP.mult, OP.add)
                        yield
                        pD = PS()
                        kb.mm(pD[:, 0:128], P[:, :], Rr[:, :])
                        dl = SQh()
                        kb.copy(dl[:, :], pD[:, 0:128], eng="act")
                        yield
                        pS = PS()
                        kb.mm(pS[:, 0:128], KE[:, :], dl[:, :])
                        if main:
                            kb.mm(pS[:, 128:256], AQKT[:, :], dl[:, :])
                            kb.mm(pS[:, 256:384], qT[:, cs], S_g[:, h, :])
                            O2 = SQh()
                            kb.copy(O2[:, :], pS[:, 128:256], eng="act")
                            o = SQh()
                            kb.stt(o[:, :], pS[:, 256:384], eg[:, c, h:h + 1], O2[:, :], OP.mult, OP.add)
                        kb.stt(S_g[:, h, :], S_g[:, h, :], cdt[:, c, h:h + 1], pS[:, 0:128], OP.mult, OP.add)
                        yield
                        if main:
                            c_ = COL()
                            j = SQh()
                            kb.act(j[:, :], o[:, :], AF.Square, accum=c_[:, 0:1])
                            rstd_from_ss(c_[:, 0:1], 128.0, c_[:, 1:2])
                            kb.stt(o[:, :], o[:, :], c_[:, 1:2], gng[:, :], OP.mult, OP.mult)
                            kb.tt(o[:, :], o[:, :], zs[:, c, :], OP.mult)
                            yield
                            pm = PS()
                            kb.tr(pm[:, 0:128], o[:, :], ident)
                            kb.act(mixedT[:, h, cs], pm[:, 0:128], AF.Identity)
                            yield

                def ret_gen():
                    for h in range(4):
                        gam = 1.0 - 2.0 ** (-5.0 - h)
                        qk = []
                        for which in range(2):
                            base = 4112 + which * 1024 + h * 256
                            if which == 0 and not main:
                                qk.append(None)
                                continue
                            raw = []
                            for kc in range(2):
                                wt_ = load_w(w_in, base + kc * 128, 128, WQ())
                                p = PS()
                                proj_fm(wt_, p[:, 0:TB])
                                r_ = FM()
                                kb.act(r_[:, :], p[:, 0:TB], AF.Identity, scale=(1.0 if which == 0 else 1.0 / 16.0))
                                raw.append(r_)
                                yield
                            a_, b_, c2_, d_ = FM(), FM(), FM(), FM()
                            kb.tt(a_[:, :], raw[0][:, :], cosT[:, :], OP.mult)
                            kb.tt(b_[:, :], raw[1][:, :], sinT[:, :], OP.mult, eng="pool")
                            kb.tt(c2_[:, :], raw[1][:, :], cosT[:, :], OP.mult)
                            kb.tt(d_[:, :], raw[0][:, :], sinT[:, :], OP.mult, eng="pool")
                            kb.tt(a_[:, :], a_[:, :], b_[:, :], OP.subtract)
                            kb.tt(c2_[:, :], c2_[:, :], d_[:, :], OP.add, eng="pool")
                            qk.append((a_, c2_))
                            yield
                        rq, rk = qk
                        kd = tk[0]
                        for kc in range(2):
                            p = PS()
                            for c in range(NCH):
                                kb.tr(p[:, c * 128:(c + 1) * 128], rk[kc][:, c * 128:(c + 1) * 128], ident)
                            kb.act(kd[:, :, kc * 128:(kc + 1) * 128], r3(p[:, 0:TB], NCH), AF.Identity, scale=RKD(h))
                            yield
                        vt = tk[1]
                        wv = load_w(w_in, 4112 + 2048 + h * 256, 256, WB1())
                        for tt in range(NCH):
                            p = PS()
                            proj_tm(wv, 256, tt, p[:, 0:256])
                            kb.act(vt[:, tt, :], p[:, 0:256], AF.Identity)
                        yield
                        if main:
                            gt_ = tk[3]
                            wg = load_w(w_in, 4112 + 3072 + h * 256, 256, WB1())
                            for tt in range(NCH):
                                p = PS()
                                proj_tm(wg, 256, tt, p[:, 0:256])
                                kb.act(gt_[:, tt, :], p[:, 0:256], AF.Silu)
                            yield
                        for c in range(NCH):
                            cs = slice(c * 128, (c + 1) * 128)
                            if main:
                                pSc = PS()
                                for kc in range(2):
                                    kb.mm(pSc[:, 0:128], rk[kc][:, cs], rq[kc][:, cs], start=(kc == 0), stop=(kc == 1))
                                SCT = SQ()
                                kb.tt(SCT[:, :], pSc[:, 0:128], RDT(h), OP.mult)
                                yield
                                pI = PS()
                                kb.mm(pI[:, 0:256], SCT[:, :], vt[:, c, :])
                                for kc in range(2):
                                    kb.mm(pI[:, 256:512], rq[kc][:, cs], S_r[:, h * 2 + kc, :], start=(kc == 0), stop=(kc == 1))
                                Isb = O256()
                                kb.copy(Isb[:, :], pI[:, 0:256], eng="act")
                                o = O256()
                                kb.stt(o[:, :], pI[:, 256:512], RQD(h), Isb[:, :], OP.mult, OP.add)
                                yield
                            pS = PS()
                            for kc in range(2):
                                kb.mm(pS[:, kc * 256:(kc + 1) * 256], kd[:, c, kc * 128:(kc + 1) * 128], vt[:, c, :])
                            for kc in range(2):
                                kb.stt(S_r[:, h * 2 + kc, :], S_r[:, h * 2 + kc, :], float(gam ** 128),
                                       pS[:, kc * 256:(kc + 1) * 256], OP.mult, OP.add)
                            yield
                            if main:
                                c_ = COL()
                                j = O256()
                                kb.act(j[:, :], o[:, :], AF.Identity, accum=c_[:, 0:1])
                                kb.act(j[:, :], o[:, :], AF.Square, accum=c_[:, 1:2])
                                kb.ts(c_[:, 2:3], c_[:, 0:1], 1.0 / 256.0, OP.mult)
                                kb.tt(c_[:, 3:4], c_[:, 2:3], c_[:, 2:3], OP.mult)
                                kb.stt(c_[:, 4:5], c_[:, 1:2], 1.0 / 256.0, c_[:, 3:4], OP.mult, OP.subtract)
                                kb.ts(c_[:, 4:5], c_[:, 4:5], EPS, OP.add)
                                kb.act(c_[:, 4:5], c_[:, 4:5], AF.Sqrt)
                                kb.recip(c_[:, 5:6], c_[:, 4:5])
                                kb.ts(o[:, :], o[:, :], c_[:, 2:3], OP.subtract, c_[:, 5:6], OP.mult)
                                kb.tt(o[:, :], o[:, :], rng[:, h * 256:(h + 1) * 256], OP.mult)
                                kb.tt(o[:, :], o[:, :], rnb[:, h * 256:(h + 1) * 256], OP.add, eng="pool")
                                kb.tt(o[:, :], o[:, :], gt_[:, c, :], OP.mult)
                                yield
                                pm = PS()
                                for kc in range(2):
                                    kb.tr(pm[:, kc * 128:(kc + 1) * 128], o[:, kc * 128:(kc + 1) * 128], ident)
                                kb.act(mixedT[:, 8 + h * 2: 10 + h * 2, cs], r3(pm[:, 0:256], 2), AF.Identity)
                                yield


                rgen = [ret_gen()]

                def step_ret():
                    if rgen[0] is not None:
                        try:
                            next(rgen[0])
                        except StopIteration:
                            rgen[0] = None

                for hg in range(2):
                    gens = []
                    for sl in range(4):
                        gdn_pre(hg * 4 + sl, sl)
                        gens.append(gdn_chunks(hg * 4 + sl, sl))
                    while gens:
                        for g_ in list(gens):
                            try:
                                next(g_)
                            except StopIteration:
                                gens.remove(g_)
                        step_ret()
                while rgen[0] is not None:
                    step_ret()

                CHK('gdn')
                CHK('ret')
                if blk == NBLK - 1:
                    kb.ts(S_g[:, :, :], S_g[:, :, :], flag[:, 0:1], OP.mult)
                    kb.ts(S_r[:, :, :], S_r[:, :, :], flag[:, 0:1], OP.mult)
                    kb.ts(carry[:, :, :], carry[:, :, :], flag[:, 0:1], OP.mult)
                if not main:
                    continue
                bi = t0 // TG
                h2T = hT
                for tt in range(NCH):
                    x_ = xt[tt % 2]
                    r0 = t0 + tt * 128
                    kb.dma("sp", x_[:, :], DV(xm[r0: r0 + 128, :]))
                    for dg in range(8):
                        wo = load_w(w_out, dg * 256, 256, WB1())
                        p = PS()
                        for k in range(16):
                            kb.mm(p[:, 0:256], mixedT[:, k, tt * 128:(tt + 1) * 128], wo[:, k, :], start=(k == 0), stop=(k == 15))
                        dsl = slice(dg * 256, (dg + 1) * 256)
                        tmp_ = FM()
                        kb.tt(tmp_[:, :], p[:, 0:256], gt1row[:, dsl], OP.mult)
                        kb.tt(x_[:, dsl], x_[:, dsl], tmp_[:, :], OP.add, eng="pool")
                    kb.dma("sp", View(x2s[bi], x2s_ap[r0: r0 + 128, :]), x_[:, :])
                    norm_mod_T(x_, 32, 48, h2T, tt, f32T=h2f)
                    p = PS()
                    for k in range(16):
                        kb.mm(p[:, 0:32], h2f[:, k, :], wr[:, k, :], start=(k == 0), stop=(k == 15))
                    lg = small
                    kb.tt(lg[:, 0:32], p[:, 0:32], br[:, :], OP.add)
                    c_ = COL()
                    cv_, lv_, lm_ = c_[:, 0:8], lg[:, 0:32], lg[:, 32:64]
                    kb.op("dve", lambda: nc.vector.max(out=cv_.ap, in_=lv_.ap), [lg], [c_])
                    kb.ts(lg[:, 32:64], lg[:, 0:32], c_[:, 3:4], OP.is_ge)
                    kb.ts(lg[:, 0:32], lg[:, 0:32], c_[:, 0:1], OP.subtract)
                    kb.act(lg[:, 0:32], lg[:, 0:32], AF.Exp)
                    kb.tt(lg[:, 0:32], lg[:, 0:32], lg[:, 32:64], OP.mult)
                    c2 = COL()
                    c2v = c2[:, 0:1]
                    kb.op("dve", lambda: nc.vector.tensor_reduce(out=c2v.ap, in_=lv_.ap,
                                                                 axis=mybir.AxisListType.X, op=OP.add), [lg], [c2])
                    kb.recip(c2[:, 1:2], c2[:, 0:1])
                    kb.ts(lg[:, 0:32], lg[:, 0:32], c2[:, 1:2], OP.mult)
                    kb.dma("sp", View(gts[bi], gts_ap[r0: r0 + 128, :]), lg[:, 0:32])
                kb.dma("sp", View(h2s[bi], h2s_ap.rearrange("p (k t) -> p k t", k=16)[:, :, t0:t0 + TB]), h2T[:, :, :])
            kb.barrier()

        with contextlib.ExitStack() as st2:
            fng = kb.sb("fng", [128, D], F32, st2)
            kb.dma("sp", fng[:, :], DV(fng_d[:, :]))
            b1t = kb.sb("b1t", [128, E, 32], F32, st2)
            kb.dma("sp", b1t[:, :, :], DV(b1_d[:, :].rearrange("p (e f) -> p e f", f=32)))
            kb.ts(b1t[:, :, 16:32], b1t[:, :, 16:32], 1.0, OP.add)
            b2t = kb.sb("b2t", [E, D], F32, st2)
            kb.dma("sp", b2t[:, :], DV(b2_d[:, :]))
            h2 = kb.sb("h2", [128, 16, TG], BF16, st2)
            gate = kb.sb("gate", [128, 4, E], F32, st2)
            gateT = kb.sb("gateT", [E, TG], F32, st2)
            acc = [kb.sb("acc%d" % i, [128, D], F32, st2) for i in range(4)]
            actT = [kb.sb("actT%d" % i, [128, 16, TG], BF16, st2) for i in range(1)]
            NEW = 6
            ew = [kb.sb("ew%d" % i, [128, TG], F32, st2) for i in range(NEW)]
            ewi = [0]

            def EW():
                ewi[0] = (ewi[0] + 1) % NEW
                return ew[ewi[0]]
            wb2 = [kb.sb("wb2_%d" % i, [128, 16, 512], BF16, st2) for i in range(3)]
            wb2i = [0]

            def WB2():
                wb2i[0] = (wb2i[0] + 1) % 3
                return wb2[wb2i[0]]
            x2t = [kb.sb("x2t%d" % i, [128, D], F32, st2) for i in range(1)]
            colm = [kb.sb("colm%d" % i, [128, 4], F32, st2) for i in range(4)]
            for tg in range(NTG if do_moe else 0):
                t0 = tg * TG
                kb.dma("sp", h2[:, :, :], View(h2s[tg], h2s_ap.rearrange("p (k t) -> p k t", k=16)[:, :, t0:t0 + TG]))
                kb.dma("sp", gate[:, :, :], View(gts[tg], gts_ap[t0:t0 + TG, :].rearrange("(c p) e -> p c e", p=128)))
                p = PS()
                for tt in range(4):
                    kb.tr(p[0:E, tt * 128:(tt + 1) * 128], gate[:, tt, :], ident)
                kb.copy(gateT[:, :], p[0:E, :])
                for tt in range(4):
                    for dg in range(4):
                        p = PS()
                        kb.mm(p[:, :], gateT[:, tt * 128:(tt + 1) * 128], b2t[:, dg * 512:(dg + 1) * 512])
                        kb.copy(acc[tt][:, dg * 512:(dg + 1) * 512], p[:, :], eng="act")
                for e in range(n_exp):
                    kb.maybe_roll()
                    aT = actT[0]
                    for g in range(4):
                        wg_ = load_w(w1[e], g * 512, 512, WB2())
                        wl_ = load_w(w1[e], D + g * 512, 512, WB2())
                        for j in range(4):
                            fj = g * 4 + j
                            pg = PS()
                            pl = PS()
                            for k in range(16):
                                kb.mm(pg[:, :], wg_[:, k, j * 128:(j + 1) * 128], h2[:, k, :], start=(k == 0), stop=(k == 15))
                            for k in range(16):
                                kb.mm(pl[:, :], wl_[:, k, j * 128:(j + 1) * 128], h2[:, k, :], start=(k == 0), stop=(k == 15))
                            gl, sg, tl = EW(), EW(), EW()
                            kb.ts(gl[:, :], pg[:, :], b1t[:, e, fj:fj + 1], OP.add, 7.0, OP.min)
                            kb.act(sg[:, :], gl[:, :], AF.Sigmoid, scale=1.702)
                            kb.ts(tl[:, :], pl[:, :], b1t[:, e, 16 + fj:17 + fj], OP.add, -6.0, OP.max)
                            kb.tt(sg[:, :], sg[:, :], gl[:, :], OP.mult, eng="pool")
                            kb.stt(aT[:, fj, :], tl[:, :], 8.0, sg[:, :], OP.min, OP.mult)
                    for dg in range(4):
                        w2_ = load_w(w2[e], dg * 512, 512, WB2())
                        for tt in range(4):
                            p = PS()
                            for k in range(16):
                                kb.mm(p[:, :], aT[:, k, tt * 128:(tt + 1) * 128], w2_[:, k, :], start=(k == 0), stop=(k == 15))
                            dsl = slice(dg * 512, (dg + 1) * 512)
                            kb.stt(acc[tt][:, dsl], p[:, :], gate[:, tt, e:e + 1], acc[tt][:, dsl], OP.mult, OP.add)
                for tt in range(4):
                    x_ = x2t[0]
                    r0 = t0 + tt * 128
                    kb.dma("sp", x_[:, :], View(x2s[tg], x2s_ap[r0: r0 + 128, :]))
                    kb.tt(acc[tt][:, :], acc[tt][:, :], gt2row[:, :], OP.mult)
                    kb.tt(x_[:, :], x_[:, :], acc[tt][:, :], OP.add, eng="pool")
                    c_ = colm[tt]
                    kb.act(acc[tt][:, :], x_[:, :], AF.Square, accum=c_[:, 0:1])
                    kb.ts(c_[:, 1:2], c_[:, 0:1], 1.0 / D, OP.mult, EPS, OP.add)
                    kb.act(c_[:, 1:2], c_[:, 1:2], AF.Sqrt)
                    kb.recip(c_[:, 1:2], c_[:, 1:2])
                    kb.stt(x_[:, :], x_[:, :], c_[:, 1:2], fng[:, :], OP.mult, OP.mult)
                    kb.dma("sp", DV(out_d[r0: r0 + 128, :]), x_[:, :])
            kb.barrier()
      except _Stop:
        kb.barrier()
    return nc


h2f_keep = [None]


def make_consts():
    c = np.zeros((128, 1536), np.float32)
    i = np.arange(128)
    c[:, 0:128] = np.eye(128)
    c[:, 128:256] = (i[:, None] <= i[None, :])
    c[:, 256:384] = (i[None, :] < i[:, None])
    c[:, 384:512] = (i[None, :] <= i[:, None])
    c[:, 512:640] = 1.0
    for h in range(4):
        lg = np.log1p(-np.exp2(-5.0 - h))
        d = (i[None, :] - i[:, None]).astype(np.float64)
        m = np.where(d >= 0, np.exp(d * lg), 0.0)
        c[:, 640 + h * 128: 640 + (h + 1) * 128] = m
        c[:, 1152 + h] = np.exp((i + 1.0) * lg)
        c[:, 1156 + h] = np.exp((127.0 - i) * lg)
    c[:, 1160] = np.power(np.float32(10000.0), -np.linspace(0.0, 1.0, 128, dtype=np.float32)).astype(np.float32)
    return c


_NC = [None]


def kernel(x, c, positions, ada_w, ada_b, norm1_g, w_in, conv_w, gdn_a_log, gdn_dt_bias, gdn_norm_g,
           ret_norm_g, ret_norm_b, w_out, norm2_g, w_router, b_router, w1, b1, w2, b2, final_norm_g):
    f = np.float32
    x = np.asarray(x, f)

    def rep(v, n=128):
        v = np.asarray(v, f).reshape(1, -1)
        return np.ascontiguousarray(np.broadcast_to(v, (n, v.shape[1])))

    def pk(v):
        return np.ascontiguousarray(np.asarray(v, f).reshape(-1, 128).T)
    shared = {
        "ada_w": np.ascontiguousarray(np.asarray(ada_w, f)[0]),
        "ada_b": rep(ada_b[0]),
        "n1g": rep(norm1_g[0]), "n2g": rep(norm2_g[0]), "fng": rep(final_norm_g),
        "w_in": np.ascontiguousarray(np.asarray(w_in, f)[0]),
        "convw": np.ascontiguousarray(np.asarray(conv_w, f)[0].T.reshape(24, 128, 4).transpose(1, 0, 2).reshape(128, 96)),
        "alog": rep(gdn_a_log[0]), "dtb": rep(gdn_dt_bias[0]), "gng": rep(gdn_norm_g[0]),
        "rng": rep(ret_norm_g[0]), "rnb": rep(ret_norm_b[0]),
        "w_out": np.ascontiguousarray(np.asarray(w_out, f)[0]),
        "wr": np.ascontiguousarray(np.asarray(w_router, f)[0].reshape(16, 128, 32).transpose(1, 0, 2).reshape(128, 512)),
        "br": rep(b_router[0]),
        "w1": np.ascontiguousarray(np.asarray(w1, f)[0]),
        "b1": np.ascontiguousarray(np.asarray(b1, f)[0].reshape(E, 32, 128).transpose(2, 0, 1).reshape(128, E * 32)),
        "w2": np.ascontiguousarray(np.asarray(w2, f)[0]),
        "b2": np.ascontiguousarray(np.asarray(b2, f)[0]),
        "consts": make_consts(),
    }
    pos = np.asarray(positions, np.int32)
    in_maps = []
    for core in range(8):
        b, half = core // 2, core % 2
        m = dict(shared)
        m["xm"] = np.ascontiguousarray(x[b, half * NTOK:(half + 1) * NTOK])
        m["xp"] = np.ascontiguousarray(x[b, 0:NTOK])
        m["posm"] = np.ascontiguousarray(np.broadcast_to(pos[b, half * NTOK:(half + 1) * NTOK][None, :], (128, NTOK)))
        m["posp"] = np.ascontiguousarray(np.broadcast_to(pos[b, 0:NTOK][None, :], (128, NTOK)))
        m["flag"] = np.full((128, 1), float(half), f)
        m["c"] = pk(np.asarray(c, f)[b])
        in_maps.append(m)
    if _NC[0] is None:
        _NC[0] = build_program()
    res = run_bass_kernel_spmd(_NC[0], in_maps, core_ids=list(range(8)))
    out = np.zeros((4, 2 * NTOK, D), f)
    for core in range(8):
        b, half = core // 2, core % 2
        out[b, half * NTOK:(half + 1) * NTOK] = res.results[core]["out"]
    return out
```

```python
import contextlib
import numpy as np
import concourse.bass as bass
import concourse.mybir as mybir
from concourse.bass_utils import run_bass_kernel_spmd

F32 = mybir.dt.float32
BF16 = mybir.dt.bfloat16
F32R = mybir.dt.float32r
I32 = mybir.dt.int32
AF = mybir.ActivationFunctionType
OP = mybir.AluOpType

D = 2048
NTOK = 2048
TB = 256
NBLK = 8
TG = 512
NTG = 4
NCH = TB // 128
E = 32
EPS = 1e-6
IN_COLS = 8208
SEM_LIM = 20000
MATH_PI = float(np.pi)


class View:
    __slots__ = ("t", "ap")

    def __init__(self, t, ap):
        self.t = t
        self.ap = ap


class T:
    def __init__(self, h, excl=False, r32=False):
        self.h = h
        self.r32 = r32
        self.lastw = None
        self.reads = {}
        self.excl = excl

    def __getitem__(self, idx):
        return View(self, self.h[idx])


class KB:
    def __init__(self, nc):
        self.nc = nc
        self.es = contextlib.ExitStack()
        self.eng = {"pe": nc.tensor, "act": nc.scalar, "dve": nc.vector, "pool": nc.gpsimd, "sp": nc.sync}
        self.cur = {}
        self.seen = {k: {} for k in self.eng}
        self.nsem = 0
        self.last_ev = {}
        self.dma_pool = {}
        self.dma_idx = {}
        self.all_dma = {}
        self.dead = False
        for k in ("pe", "act", "dve", "pool"):
            self._new_epoch(k)
        for q in ("sp", "pool"):
            self.dma_pool[q] = [[self._sem("dq_%s%d" % (q, i)), 0] for i in range(12)]
            self.dma_idx[q] = 0

    def _sem(self, name):
        self.nsem += 1
        h = self.es.enter_context(self.nc.semaphore("%s_%d" % (name, self.nsem)))
        return (self.nsem, h)

    def _new_epoch(self, e):
        self.cur[e] = [self._sem("e_" + e), 0]

    def sb(self, name, shape, dt, stack=None, r32=False):
        return T((stack or self.es).enter_context(self.nc.sbuf_tensor("s_" + name, shape, dt)), r32=r32)

    @staticmethod
    def _o(v):
        return v.ap.bitcast(F32R) if (v.t is not None and v.t.r32) else v.ap

    def _wait(self, e, need):
        seen = self.seen[e]
        own = self.cur[e][0][0] if e in self.cur else None
        for (key, h, v) in need:
            if e == "pe" and key == own:
                continue
            if seen.get(key, 0) >= v:
                continue
            self.eng[e].wait_ge(h, v)
            seen[key] = v

    def _deps(self, R, W):
        need = []
        for t in R:
            if t is not None and t.lastw is not None:
                need.append(t.lastw)
            if t is not None and t.excl:
                need.extend(t.reads.values())
        for t in W:
            if t is None:
                continue
            if t.lastw is not None:
                need.append(t.lastw)
            need.extend(t.reads.values())
        return need

    def _record(self, ev, R, W):
        for t in R:
            if t is not None:
                t.reads[ev[0]] = ev
        for t in W:
            if t is not None:
                t.lastw = ev
                t.reads = {}

    def op(self, e, fn, R, W):
        if self.dead:
            return
        self._wait(e, self._deps(R, W))
        inst = fn()
        c = self.cur[e]
        c[1] += 1
        inst.then_inc(c[0][1], 1)
        ev = (c[0][0], c[0][1], c[1])
        self.last_ev[c[0][0]] = (e, ev)
        if c[1] >= 30000:
            self._new_epoch(e)
        self._record(ev, R, W)

    def dma(self, q, out, in_, **kw):
        if self.dead:
            return None
        R = [in_.t]
        W = [out.t]
        pool = self.dma_pool[q]
        i = self.dma_idx[q]
        self.dma_idx[q] = (i + 1) % len(pool)
        slot = pool[i]
        need = self._deps(R, W)
        if slot[1] > 0:
            need.append((slot[0][0], slot[0][1], slot[1]))
        self._wait(q, need)
        inst = self.eng[q].dma_start(out=out.ap, in_=in_.ap, **kw)
        slot[1] += 16
        if slot[1] >= SEM_LIM:
            pool[i] = [self._sem("dq_%s" % q), 0]
            slot2 = pool[i]
            slot2[1] = 16
            inst.then_inc(slot2[0][1], 16)
            ev = (slot2[0][0], slot2[0][1], 16)
        else:
            inst.then_inc(slot[0][1], 16)
            ev = (slot[0][0], slot[0][1], slot[1])
        self.all_dma[ev[0]] = ev
        self._record(ev, R, W)
        return ev

    def maybe_roll(self, lim=8000):
        if self.dead:
            return
        for e in list(self.cur):
            if self.cur[e][1] >= lim:
                self._new_epoch(e)

    def barrier(self):
        if self.dead:
            return
        evs = [ev for (_, ev) in self.last_ev.values()] + list(self.all_dma.values())
        for e in self.eng:
            self._wait_all(e, evs)

    def _wait_all(self, e, evs):
        seen = self.seen[e]
        for (key, h, v) in evs:
            if seen.get(key, 0) >= v:
                continue
            self.eng[e].wait_ge(h, v)
            seen[key] = v

    def mm(self, out, lhsT, rhs, start=True, stop=True, r=False):
        la, ra = lhsT.ap, rhs.ap
        if r and lhsT.t.r32 and rhs.t.r32:
            la, ra = la.bitcast(F32R), ra.bitcast(F32R)
        self.op("pe", lambda: self.nc.tensor.matmul(out.ap, lhsT=la, rhs=ra, start=start, stop=stop),
                [lhsT.t, rhs.t], [out.t])

    def tr(self, out, in_, ident):
        self.op("pe", lambda: self.nc.tensor.transpose(out.ap, in_.ap, ident.ap), [in_.t, ident.t], [out.t])

    def act(self, out, in_, func, bias=None, scale=None, accum=None):
        R = [in_.t]
        W = [out.t]
        kw = {}
        if bias is not None:
            if isinstance(bias, View):
                R.append(bias.t)
                kw["bias"] = bias.ap
            else:
                kw["bias"] = float(bias)
        if scale is not None:
            if isinstance(scale, View):
                R.append(scale.t)
                kw["scale"] = scale.ap
            else:
                kw["scale"] = float(scale)
        if accum is not None:
            W.append(accum.t)
            kw["accum_out"] = accum.ap
        self.op("act", lambda: self.nc.scalar.activation(out=self._o(out), in_=in_.ap, func=func, **kw), R, W)

    def ts(self, out, in0, s1, op0, s2=None, op1=None, eng="dve"):
        R = [in0.t]

        def cv(s):
            if isinstance(s, View):
                R.append(s.t)
                return s.ap
            return None if s is None else float(s)
        a1 = cv(s1)
        a2 = cv(s2)
        kw = {}
        if op1 is not None:
            kw["op1"] = op1
        E_ = self.eng[eng]
        self.op(eng, lambda: E_.tensor_scalar(out=self._o(out), in0=in0.ap, scalar1=a1, scalar2=a2, op0=op0, **kw),
                R, [out.t])

    def tt(self, out, in0, in1, op, eng="dve"):
        E_ = self.eng[eng]
        self.op(eng, lambda: E_.tensor_tensor(out=self._o(out), in0=in0.ap, in1=in1.ap, op=op),
                [in0.t, in1.t], [out.t])

    def stt(self, out, in0, scalar, in1, op0, op1):
        R = [in0.t, in1.t]
        if isinstance(scalar, View):
            R.append(scalar.t)
            s = scalar.ap
        else:
            s = float(scalar)
        self.op("dve", lambda: self.nc.vector.scalar_tensor_tensor(out=self._o(out), in0=in0.ap, scalar=s, in1=in1.ap,
                                                                   op0=op0, op1=op1), R, [out.t])

    def copy(self, out, in_, eng="dve"):
        if eng == "act":
            self.act(out, in_, AF.Identity)
        else:
            E_ = self.eng[eng]
            self.op(eng, lambda: E_.tensor_copy(out=self._o(out), in_=in_.ap), [in_.t], [out.t])

    def memset(self, out, val, eng="dve"):
        E_ = self.eng[eng]
        self.op(eng, lambda: E_.memset(out.ap, val), [], [out.t])

    def recip(self, out, in_):
        self.op("dve", lambda: self.nc.vector.reciprocal(out=out.ap, in_=in_.ap), [in_.t], [out.t])


class _Stop(Exception):
    pass


def build_program(n_exp=E, dbg=False, do_moe=True, stop=None):
    nc = bass.Bass("TRN2", target_bir_lowering=False)
    kb = KB(nc)

    def dram_in(name, shape, dt=F32):
        return nc.dram_tensor(name, shape, dt, kind="ExternalInput").ap()

    xm = dram_in("xm", [NTOK, D])
    xp = dram_in("xp", [NTOK, D])
    posm = dram_in("posm", [128, NTOK], I32)
    posp = dram_in("posp", [128, NTOK], I32)
    flag_d = dram_in("flag", [128, 1])
    c_d = dram_in("c", [128, 16])
    ada_w = dram_in("ada_w", [D, 6 * D])
    ada_b_d = dram_in("ada_b", [128, 6 * D])
    n1g_d = dram_in("n1g", [128, D])
    n2g_d = dram_in("n2g", [128, D])
    fng_d = dram_in("fng", [128, D])
    w_in = dram_in("w_in", [D, IN_COLS])
    convw_d = dram_in("convw", [128, 24 * 4])
    alog_d = dram_in("alog", [128, 8])
    dtb_d = dram_in("dtb", [128, 8])
    gng_d = dram_in("gng", [128, 128])
    rng_d = dram_in("rng", [128, 1024])
    rnb_d = dram_in("rnb", [128, 1024])
    w_out = dram_in("w_out", [D, D])
    wr_d = dram_in("wr", [128, 16 * 32])
    br_d = dram_in("br", [128, 32])
    w1 = dram_in("w1", [n_exp, D, 2 * D])
    b1_d = dram_in("b1", [128, E * 32])
    w2 = dram_in("w2", [n_exp, D, D])
    b2_d = dram_in("b2", [E, D])
    consts_d = dram_in("consts", [128, 1536])
    out_d = nc.dram_tensor("out", [NTOK, D], F32, kind="ExternalOutput").ap()
    ik = "ExternalOutput" if dbg else "Internal"
    x2s_ap = nc.dram_tensor("x2s", [NTOK, D], F32, kind=ik).ap()
    h2s_ap = nc.dram_tensor("h2s", [128, 16 * NTOK], BF16, kind=ik).ap()
    gts_ap = nc.dram_tensor("gts", [NTOK, E], F32, kind=ik).ap()
    x2s = [T(None) for _ in range(NTG)]
    h2s = [T(None) for _ in range(NTG)]
    gts = [T(None) for _ in range(NTG)]

    def DV(ap):
        return View(None, ap)

    def r3(v, j):
        return View(v.t, v.ap.rearrange("p (j t) -> p j t", j=j))

    def CHK(tag):
        if stop == tag and not kb.dead:
            kb.barrier()
            kb.dead = True

    with kb.es:
      try:
        cst = kb.sb("cst", [128, 1536], F32)
        ident = cst[:, 0:128]
        TRI = cst[:, 128:256]
        MLS = cst[:, 256:384]
        MLI = cst[:, 384:512]
        ONES = cst[:, 512:640]

        def RDT(h):
            return cst[:, 640 + h * 128: 640 + (h + 1) * 128]

        def RQD(h):
            return cst[:, 1152 + h: 1153 + h]

        def RKD(h):
            return cst[:, 1156 + h: 1157 + h]
        INVF = cst[:, 1160:1161]
        kb.dma("sp", cst[:, :], DV(consts_d[:, :]))
        flag = kb.sb("flag", [128, 1], F32)
        kb.dma("sp", flag[:, :], DV(flag_d[:, :]))
        gt1row = kb.sb("gt1row", [128, D], F32)
        gt2row = kb.sb("gt2row", [128, D], F32)
        modc = kb.sb("modc", [128, 64], F32)
        ps = [T(kb.es.enter_context(nc.psum_tensor("ps%d" % i, [128, 512], F32)), excl=True) for i in range(8)]
        psi = [0]

        def PS():
            psi[0] = (psi[0] + 1) % 8
            return ps[psi[0]]

        def load_w(src2d, c0, ncols, dst):
            kb.dma("pool", dst[:, :, 0:ncols], DV(src2d[:, c0:c0 + ncols].rearrange("(k p) c -> p k c", p=128)))
            return dst

        with contextlib.ExitStack() as st0:
            cond = kb.sb("cond", [128, 16], F32, st0)
            kb.dma("sp", cond[:, :], DV(c_d[:, :]))
            kb.act(cond[:, :], cond[:, :], AF.Silu)
            condb = kb.sb("condb", [128, 16, 128], F32, st0)
            for k in range(16):
                kb.ts(condb[:, k, :], ONES, cond[:, k:k + 1], OP.mult)
            adab = kb.sb("adab", [128, 6 * D], F32, st0)
            kb.dma("sp", adab[:, :], DV(ada_b_d[:, :]))
            aw = [kb.sb("aw%d" % i, [128, 16, 512], F32, st0) for i in range(2)]
            modr = kb.sb("modr", [128, 6, D], F32, st0)
            for g in range(24):
                a = aw[g % 2]
                kb.dma("sp", a[:, :, :], DV(ada_w[:, g * 512:(g + 1) * 512].rearrange("(k p) c -> p k c", p=128)))
                p = PS()
                for k in range(16):
                    kb.mm(p[:, :], condb[:, k, :], a[:, k, :], start=(k == 0), stop=(k == 15))
                kb.tt(modr[:, g // 4, (g % 4) * 512:(g % 4 + 1) * 512], p[:, :], adab[:, g * 512:(g + 1) * 512], OP.add)
            tmpg = kb.sb("tmpg", [128, D], F32, st0)
            for (src, gd) in ((1, n1g_d), (4, n2g_d)):
                kb.dma("sp", tmpg[:, :], DV(gd[:, :]))
                kb.stt(modr[:, src, :], modr[:, src, :], 1.0, tmpg[:, :], OP.add, OP.mult)
            kb.copy(gt1row[:, :], modr[:, 2, :])
            kb.copy(gt2row[:, :], modr[:, 5, :], eng="pool")
            for vi, src in enumerate((1, 0, 4, 3)):
                for g in range(4):
                    p = PS()
                    for j in range(4):
                        kc = g * 4 + j
                        kb.tr(p[:, j * 128:(j + 1) * 128], modr[:, src, kc * 128:(kc + 1) * 128], ident)
                    for j in range(4):
                        kc = g * 4 + j
                        kb.copy(modc[:, vi * 16 + kc: vi * 16 + kc + 1], p[:, j * 128: j * 128 + 1])
            kb.barrier()
        CHK('p0')

        with contextlib.ExitStack() as st1:
            S_g = kb.sb("S_g", [128, 8, 128], F32, st1, r32=True)
            S_r = kb.sb("S_r", [128, 8, 256], F32, st1)
            carry = kb.sb("carry", [128, 24, 4], F32, st1)
            kb.memset(S_g[:, :, :], 0.0)
            kb.ts(S_g[:, :, :], S_g[:, :, :], 1.0, OP.mult)
            kb.memset(S_r[:, :, :], 0.0)
            kb.memset(carry[:, :, :], 0.0)
            convw = kb.sb("convw", [128, 24, 4], F32, st1)
            kb.dma("sp", convw[:, :, :], DV(convw_d[:, :].rearrange("p (c i) -> p c i", i=4)))
            negA = kb.sb("negA", [128, 8], F32, st1)
            dtb = kb.sb("dtb", [128, 8], F32, st1)
            kb.dma("sp", negA[:, :], DV(alog_d[:, :]))
            kb.dma("sp", dtb[:, :], DV(dtb_d[:, :]))
            kb.act(negA[:, :], negA[:, :], AF.Exp)
            kb.ts(negA[:, :], negA[:, :], -1.0, OP.mult)
            gng = kb.sb("gng", [128, 128], F32, st1)
            kb.dma("sp", gng[:, :], DV(gng_d[:, :]))
            rng = kb.sb("rng", [128, 1024], F32, st1)
            rnb = kb.sb("rnb", [128, 1024], F32, st1)
            kb.dma("sp", rng[:, :], DV(rng_d[:, :]))
            kb.dma("sp", rnb[:, :], DV(rnb_d[:, :]))
            wr = kb.sb("wr", [128, 16, 32], F32, st1)
            kb.dma("sp", wr[:, :, :], DV(wr_d[:, :].rearrange("p (k e) -> p k e", e=32)))
            br = kb.sb("br", [128, 32], F32, st1)
            kb.dma("sp", br[:, :], DV(br_d[:, :]))

            xt = [kb.sb("xt%d" % i, [128, D], F32, st1) for i in range(2)]
            hT = kb.sb("hT", [128, 16, TB], BF16, st1)
            mixedT = kb.sb("mixedT", [128, 16, TB], BF16, st1)
            h2f = kb.sb("h2f", [128, 16, 128], F32, st1)
            small = kb.sb("small", [128, 64], F32, st1)
            ba = kb.sb("ba", [128, NCH, 16], F32, st1)
            beta = kb.sb("beta", [128, NCH, 8], F32, st1)
            gs_ = kb.sb("gs", [128, NCH, 8], F32, st1)
            gcol = kb.sb("gcol", [128, NCH, 8], F32, st1)
            eg = kb.sb("eg", [128, NCH, 8], F32, st1)
            nbeg = kb.sb("nbeg", [128, NCH, 8], F32, st1)
            kes = kb.sb("kes", [128, NCH, 8], F32, st1)
            cdt = kb.sb("cd", [128, NCH, 8], F32, st1)
            tmp8 = kb.sb("tmp8", [128, NCH, 8], F32, st1)
            tmp8b = kb.sb("tmp8b", [128, NCH, 8], F32, st1)
            cosT = kb.sb("cosT", [128, TB], F32, st1)
            sinT = kb.sb("sinT", [128, TB], F32, st1)
            posi = kb.sb("posi", [128, TB], I32, st1)
            uw = [kb.sb("uw%d" % i, [128, TB + 3], F32, st1) for i in range(2)]
            NFM = 10
            fm = [kb.sb("fm%d" % i, [128, TB], F32, st1) for i in range(NFM)]
            fmi = [0]

            def FM():
                fmi[0] = (fmi[0] + 1) % NFM
                return fm[fmi[0]]
            slot_q = [kb.sb("slq%d" % i, [128, TB], F32, st1, r32=True) for i in range(4)]
            slot_k = [kb.sb("slk%d" % i, [128, TB], F32, st1, r32=True) for i in range(4)]
            slot_kt = [kb.sb("slkt%d" % i, [128, NCH, 128], F32, st1) for i in range(4)]
            slot_vt = [kb.sb("slvt%d" % i, [128, NCH, 128], F32, st1) for i in range(4)]
            slot_z = [kb.sb("slz%d" % i, [128, NCH, 128], F32, st1) for i in range(4)]
            slot_aq = [kb.sb("slaq%d" % i, [128, 128], F32, st1, r32=True) for i in range(4)]
            slot_sq = [[kb.sb("slsq%d_%d" % (i, j), [128, 128], F32, st1, r32=True) for j in range(10)] for i in range(4)]
            nrp = [kb.sb("nrp%d" % i, [128, 128], F32, st1) for i in range(8)]
            nri = [0]

            def NR():
                nri[0] = (nri[0] + 1) % 8
                return nrp[nri[0]]
            NSQ = 4
            sq = [kb.sb("sq%d" % i, [128, 128], F32, st1) for i in range(NSQ)]
            sqi = [0]

            def SQ():
                sqi[0] = (sqi[0] + 1) % NSQ
                return sq[sqi[0]]
            tk = [kb.sb("tk%d" % i, [128, NCH, 256], F32, st1) for i in range(4)]
            wq = [kb.sb("wq%d" % i, [128, 16, 128], BF16, st1) for i in range(3)]
            wqi = [0]

            def WQ():
                wqi[0] = (wqi[0] + 1) % 3
                return wq[wqi[0]]
            wb1 = [kb.sb("wb1_%d" % i, [128, 16, 256], BF16, st1) for i in range(2)]
            wb1i = [0]

            def WB1():
                wb1i[0] = (wb1i[0] + 1) % 2
                return wb1[wb1i[0]]
            col = [kb.sb("col%d" % i, [128, 8], F32, st1) for i in range(8)]
            coli = [0]

            def COL():
                coli[0] = (coli[0] + 1) % 8
                return col[coli[0]]
            o256 = [kb.sb("o256_%d" % i, [128, 256], F32, st1) for i in range(4)]
            o2i = [0]

            def O256():
                o2i[0] = (o2i[0] + 1) % 4
                return o256[o2i[0]]

            def rstd_from_ss(ss, n, dst):
                kb.ts(dst, ss, 1.0 / n, OP.mult, EPS, OP.add)
                kb.act(dst, dst, AF.Sqrt)
                kb.recip(dst, dst)

            def norm_mod_T(xtile, gbase, sbase, dstT, tt, f32T=None):
                c_ = COL()
                j_ = FM()
                for q4 in range(D // TB):
                    kb.act(j_[:, :], xtile[:, q4 * TB:(q4 + 1) * TB], AF.Square, accum=c_[:, 2 + (q4 % 4):3 + (q4 % 4)]) \
                        if False else None
                kb.act(xtile_junk[:, :], xtile[:, :], AF.Square, accum=c_[:, 0:1])
                rstd_from_ss(c_[:, 0:1], float(D), c_[:, 1:2])
                kb.ts(xtile[:, :], xtile[:, :], c_[:, 1:2], OP.mult)
                for g in range(4):
                    p = PS()
                    for j in range(4):
                        kc = g * 4 + j
                        kb.tr(p[:, j * 128:(j + 1) * 128], xtile[:, kc * 128:(kc + 1) * 128], ident)
                    for j in range(4):
                        kc = g * 4 + j
                        kb.act(dstT[:, kc, tt * 128:(tt + 1) * 128], p[:, j * 128:(j + 1) * 128], AF.Identity,
                               scale=modc[:, gbase + kc: gbase + kc + 1], bias=modc[:, sbase + kc: sbase + kc + 1])
                        if f32T is not None:
                            kb.ts(f32T[:, kc, :], p[:, j * 128:(j + 1) * 128], modc[:, gbase + kc: gbase + kc + 1], OP.mult,
                                  modc[:, sbase + kc: sbase + kc + 1], OP.add)

            xtile_junk = kb.sb("xjunk", [128, D], BF16, st1)

            def proj_fm(wtile, dst_ps):
                for k in range(16):
                    kb.mm(dst_ps, wtile[:, k, 0:128], hT[:, k, :], start=(k == 0), stop=(k == 15))

            def proj_tm(wtile, ncols, tt, dst_ps):
                for k in range(16):
                    kb.mm(dst_ps, hT[:, k, tt * 128:(tt + 1) * 128], wtile[:, k, 0:ncols], start=(k == 0), stop=(k == 15))

            for blk in range(2 * NBLK):
                kb.maybe_roll()
                main = blk >= NBLK
                xsrc = xm if main else xp
                psrc = posm if main else posp
                t0 = (blk % NBLK) * TB
                for tt in range(NCH):
                    x_ = xt[tt % 2]
                    kb.dma("sp", x_[:, :], DV(xsrc[t0 + tt * 128: t0 + (tt + 1) * 128, :]))
                    norm_mod_T(x_, 0, 16, hT, tt)
                CHK('norm1')
                ang, ang2 = FM(), FM()
                kb.dma("sp", posi[:, :], DV(psrc[:, t0:t0 + TB]))
                kb.copy(ang[:, :], posi[:, :])
                kb.ts(ang[:, :], ang[:, :], INVF, OP.mult)
                for (shift, dst) in ((0.0, sinT), (MATH_PI / 2, cosT)):
                    kb.ts(ang2[:, :], ang[:, :], shift, OP.add, 1.0 / (2 * MATH_PI), OP.mult)
                    kb.copy(posi[:, :], ang2[:, :])
                    kb.copy(ang2[:, :], posi[:, :])
                    kb.stt(ang2[:, :], ang2[:, :], -2 * MATH_PI, ang[:, :], OP.mult, OP.add)
                    kb.ts(ang2[:, :], ang2[:, :], shift, OP.add)
                    m_ = FM()
                    kb.ts(m_[:, :], ang2[:, :], MATH_PI, OP.is_gt)
                    kb.stt(ang2[:, :], m_[:, :], -2 * MATH_PI, ang2[:, :], OP.mult, OP.add)
                    kb.ts(m_[:, :], ang2[:, :], -MATH_PI, OP.is_lt)
                    kb.stt(ang2[:, :], m_[:, :], 2 * MATH_PI, ang2[:, :], OP.mult, OP.add)
                    kb.ts(ang2[:, :], ang2[:, :], MATH_PI, OP.min, -MATH_PI, OP.max)
                    kb.act(dst[:, :], ang2[:, :], AF.Sin)
                CHK('rot')
                wba = WQ()
                kb.dma("pool", wba[:, :, 0:16], DV(w_in[:, 4096:4112].rearrange("(k p) c -> p k c", p=128)))
                for tt in range(NCH):
                    p = PS()
                    proj_tm(wba, 16, tt, p[:, 0:16])
                    kb.copy(ba[:, tt, :], p[:, 0:16])
                kb.act(beta[:, :, :], ba[:, :, 0:8], AF.Sigmoid)
                for tt in range(NCH):
                    kb.tt(tmp8[:, tt, :], ba[:, tt, 8:16], dtb[:, :], OP.add)
                kb.stt(tmp8b[:, :, :], tmp8[:, :, :], -1.0, tmp8[:, :, :], OP.mult, OP.max)
                kb.act(tmp8b[:, :, :], tmp8b[:, :, :], AF.Exp, scale=-1.0)
                kb.act(tmp8b[:, :, :], tmp8b[:, :, :], AF.Ln, bias=1.0)
                kb.stt(tmp8[:, :, :], tmp8[:, :, :], 0.0, tmp8b[:, :, :], OP.max, OP.add)
                for tt in range(NCH):
                    kb.tt(gs_[:, tt, :], tmp8[:, tt, :], negA[:, :], OP.mult)
                for tt in range(NCH):
                    p = PS()
                    kb.mm(p[:, 0:8], TRI, gs_[:, tt, :])
                    kb.mm(p[:, 8:16], ONES, gs_[:, tt, :])
                    kb.copy(gcol[:, tt, :], p[:, 0:8])
                    kb.act(eg[:, tt, :], p[:, 0:8], AF.Exp)
                    kb.act(cdt[:, tt, :], p[:, 8:16], AF.Exp)
                    kb.tt(tmp8b[:, tt, :], p[:, 8:16], gcol[:, tt, :], OP.subtract)
                    kb.act(kes[:, tt, :], tmp8b[:, tt, :], AF.Exp)
                kb.stt(nbeg[:, :, :], beta[:, :, :], -1.0, eg[:, :, :], OP.mult, OP.mult)

                CHK('ba')
                def gdn_pre(h, sl):
                    qT, kT = slot_q[sl], slot_k[sl]
                    vT = None
                    for which in range(3):
                        cc = which * 8 + h
                        wt_ = load_w(w_in, cc * 128, 128, WQ())
                        p = PS()
                        proj_fm(wt_, p[:, 0:TB])
                        u = uw[which % 2]
                        kb.copy(u[:, 0:3], carry[:, cc, 0:3], eng="pool")
                        kb.act(u[:, 3:TB + 3], p[:, 0:TB], AF.Identity)
                        y = (qT, kT, None)[which] or FM()
                        kb.ts(y[:, :], u[:, 0:TB], convw[:, cc, 0:1], OP.mult)
                        for i in range(1, 4):
                            kb.stt(y[:, :], u[:, i:TB + i], convw[:, cc, i:i + 1], y[:, :], OP.mult, OP.add)
                        kb.copy(carry[:, cc, 0:3], u[:, TB:TB + 3], eng="pool")
                        kb.act(y[:, :], y[:, :], AF.Silu)
                        if which == 2:
                            vT = y
                    for (src, extra) in ((qT, float(np.log(128.0 ** -0.5))), (kT, 0.0)):
                        s2 = FM()
                        kb.act(s2[:, :], src[:, :], AF.Square)
                        p = PS()
                        kb.mm(p[:, 0:TB], ONES, s2[:, :])
                        kb.act(s2[:, :], p[:, 0:TB], AF.Ln, bias=EPS)
                        kb.act(s2[:, :], s2[:, :], AF.Exp, scale=-0.5, bias=extra)
                        kb.tt(src[:, :], src[:, :], s2[:, :], OP.mult)
                    for (src, dst) in ((kT, slot_kt[sl]), (vT, slot_vt[sl])):
                        p = PS()
                        for c in range(NCH):
                            kb.tr(p[:, c * 128:(c + 1) * 128], src[:, c * 128:(c + 1) * 128], ident)
                        kb.act(dst[:, :, :], r3(p[:, 0:TB], NCH), AF.Identity)
                    if main:
                        wz = load_w(w_in, 3072 + h * 128, 128, WQ())
                        for tt in range(NCH):
                            p = PS()
                            proj_tm(wz, 128, tt, p[:, 0:128])
                            kb.act(slot_z[sl][:, tt, :], p[:, 0:128], AF.Silu)

                def gdn_chunks(h, sl):
                    qT, kT, ktok, vtok, zs = slot_q[sl], slot_k[sl], slot_kt[sl], slot_vt[sl], slot_z[sl]
                    pool_ = slot_sq[sl]
                    pi = [0]

                    def SQh():
                        pi[0] = (pi[0] + 1) % len(pool_)
                        return pool_[pi[0]]
                    AQKT = slot_aq[sl]
                    for c in range(NCH):
                        cs = slice(c * 128, (c + 1) * 128)
                        bcol = beta[:, c, h:h + 1]
                        GSB = NR()
                        kb.ts(GSB[:, :], ONES, gs_[:, c, h:h + 1], OP.mult, eng="pool")
                        yield
                        pRB = PS()
                        kb.mm(pRB[:, 0:128], GSB[:, :], TRI)
                        NE = NR()
                        kb.ts(NE[:, :], pRB[:, 0:128], gcol[:, c, h:h + 1], OP.subtract, 0.0, OP.max)
                        EX = SQh()
                        kb.act(EX[:, :], NE[:, :], AF.Exp, scale=-1.0)
                        yield
                        ML = SQh()
                        kb.stt(ML[:, :], EX[:, :], bcol, MLS, OP.mult, OP.mult)
                        EXI = SQh()
                        kb.tt(EXI[:, :], EX[:, :], MLI, OP.mult, eng="pool")
                        yield
                        pG = PS()
                        kb.mm(pG[:, 0:128], kT[:, cs], kT[:, cs], r=True)
                        kb.mm(pG[:, 128:256], qT[:, cs], kT[:, cs], r=True)
                        L = SQh()
                        kb.tt(L[:, :], pG[:, 0:128], ML[:, :], OP.mult)
                        AQK = SQh()
                        kb.tt(AQK[:, :], pG[:, 128:256], EXI[:, :], OP.mult)
                        yield
                        pT = PS()
                        kb.tr(pT[:, 0:128], L[:, :], ident)
                        kb.tr(pT[:, 128:256], AQK[:, :], ident)
                        U = SQh()
                        kb.copy(U[:, :], pT[:, 0:128], eng="act")
                        kb.copy(AQKT[:, :], pT[:, 128:256], eng="act")
                        yield
                        P = SQh()
                        kb.tt(P[:, :], ident, U[:, :], OP.subtract)
                        Lp, Up = L, U
                        for k in range(1, 7):
                            pp = PS()
                            kb.mm(pp[:, 0:128], Up[:, :], Lp[:, :], r=True)
                            if k < 6:
                                kb.mm(pp[:, 128:256], Lp[:, :], Up[:, :], r=True)
                            Ln = SQh()
                            kb.copy(Ln[:, :], pp[:, 0:128], eng="act")
                            if k < 6:
                                Un = SQh()
                                kb.copy(Un[:, :], pp[:, 128:256])
                            else:
                                Un = None
                            yield
                            pq = PS()
                            kb.mm(pq[:, 0:128], Ln[:, :], P[:, :], r=True)
                            Pn = SQh()
                            kb.tt(Pn[:, :], pq[:, 0:128], P[:, :], OP.add)
                            P, Lp, Up = Pn, Ln, Un
                            yield
                        BV = SQh()
                        kb.ts(BV[:, :], vtok[:, c, :], bcol, OP.mult, eng="pool")
                        KE = SQh()
                        kb.ts(KE[:, :], ktok[:, c, :], kes[:, c, h:h + 1], OP.mult, eng="pool")
                        yield
                        pK = PS()
                        kb.mm(pK[:, 0:128], kT[:, cs], S_g[:, h, :], r=True)
                        Rr = SQh()
                        kb.stt(Rr[:, :], pK[:, 0:128], nbeg[:, c, h:h + 1], BV[:, :], OP.mult, OP.add)
                        yield
                        pD = PS()
                        kb.mm(pD[:, 0:128], P[:, :], Rr[:, :], r=True)
                        dl = SQh()
                        kb.copy(dl[:, :], pD[:, 0:128], eng="act")
                        yield
                        pS = PS()
                        kb.mm(pS[:, 0:128], KE[:, :], dl[:, :], r=True)
                        if main:
                            kb.mm(pS[:, 128:256], AQKT[:, :], dl[:, :], r=True)
                            kb.mm(pS[:, 256:384], qT[:, cs], S_g[:, h, :], r=True)
                            O2 = SQh()
                            kb.copy(O2[:, :], pS[:, 128:256], eng="act")
                            o = SQh()
                            kb.stt(o[:, :], pS[:, 256:384], eg[:, c, h:h + 1], O2[:, :], OP.mult, OP.add)
                        kb.stt(S_g[:, h, :], S_g[:, h, :], cdt[:, c, h:h + 1], pS[:, 0:128], OP.mult, OP.add)
                        yield
                        if main:
                            c_ = COL()
                            j = SQh()
                            kb.act(j[:, :], o[:, :], AF.Square, accum=c_[:, 0:1])
                            rstd_from_ss(c_[:, 0:1], 128.0, c_[:, 1:2])
                            kb.stt(o[:, :], o[:, :], c_[:, 1:2], gng[:, :], OP.mult, OP.mult)
                            kb.tt(o[:, :], o[:, :], zs[:, c, :], OP.mult)
                            yield
                            pm = PS()
                            kb.tr(pm[:, 0:128], o[:, :], ident)
                            kb.act(mixedT[:, h, cs], pm[:, 0:128], AF.Identity)
                            yield

                for hg in range(2):
                    gens = []
                    for sl in range(4):
                        gdn_pre(hg * 4 + sl, sl)
                        gens.append(gdn_chunks(hg * 4 + sl, sl))
                    while gens:
                        for g_ in list(gens):
                            try:
                                next(g_)
                            except StopIteration:
                                gens.remove(g_)

                CHK('gdn')
                for h in range(4):
                    gam = 1.0 - 2.0 ** (-5.0 - h)
                    qk = []
                    for which in range(2):
                        base = 4112 + which * 1024 + h * 256
                        if which == 0 and not main:
                            qk.append(None)
                            continue
                        raw = []
                        for kc in range(2):
                            wt_ = load_w(w_in, base + kc * 128, 128, WQ())
                            p = PS()
                            proj_fm(wt_, p[:, 0:TB])
                            r_ = FM()
                            kb.act(r_[:, :], p[:, 0:TB], AF.Identity, scale=(1.0 if which == 0 else 1.0 / 16.0))
                            raw.append(r_)
                        a_, b_, c2_, d_ = FM(), FM(), FM(), FM()
                        kb.tt(a_[:, :], raw[0][:, :], cosT[:, :], OP.mult)
                        kb.tt(b_[:, :], raw[1][:, :], sinT[:, :], OP.mult, eng="pool")
                        kb.tt(c2_[:, :], raw[1][:, :], cosT[:, :], OP.mult)
                        kb.tt(d_[:, :], raw[0][:, :], sinT[:, :], OP.mult, eng="pool")
                        kb.tt(a_[:, :], a_[:, :], b_[:, :], OP.subtract)
                        kb.tt(c2_[:, :], c2_[:, :], d_[:, :], OP.add, eng="pool")
                        qk.append((a_, c2_))
                    rq, rk = qk
                    kd = tk[0]
                    for kc in range(2):
                        p = PS()
                        for c in range(NCH):
                            kb.tr(p[:, c * 128:(c + 1) * 128], rk[kc][:, c * 128:(c + 1) * 128], ident)
                        kb.act(kd[:, :, kc * 128:(kc + 1) * 128], r3(p[:, 0:TB], NCH), AF.Identity, scale=RKD(h))
                    vt = tk[1]
                    wv = load_w(w_in, 4112 + 2048 + h * 256, 256, WB1())
                    for tt in range(NCH):
                        p = PS()
                        proj_tm(wv, 256, tt, p[:, 0:256])
                        kb.act(vt[:, tt, :], p[:, 0:256], AF.Identity)
                    if main:
                        gt_ = tk[3]
                        wg = load_w(w_in, 4112 + 3072 + h * 256, 256, WB1())
                        for tt in range(NCH):
                            p = PS()
                            proj_tm(wg, 256, tt, p[:, 0:256])
                            kb.act(gt_[:, tt, :], p[:, 0:256], AF.Silu)
                    for c in range(NCH):
                        cs = slice(c * 128, (c + 1) * 128)
                        if main:
                            pSc = PS()
                            for kc in range(2):
                                kb.mm(pSc[:, 0:128], rk[kc][:, cs], rq[kc][:, cs], start=(kc == 0), stop=(kc == 1), r=True)
                            SCT = SQ()
                            kb.tt(SCT[:, :], pSc[:, 0:128], RDT(h), OP.mult)
                            pI = PS()
                            kb.mm(pI[:, 0:256], SCT[:, :], vt[:, c, :], r=True)
                            for kc in range(2):
                                kb.mm(pI[:, 256:512], rq[kc][:, cs], S_r[:, h * 2 + kc, :], start=(kc == 0), stop=(kc == 1), r=True)
                            Isb = O256()
                            kb.copy(Isb[:, :], pI[:, 0:256], eng="act")
                            o = O256()
                            kb.stt(o[:, :], pI[:, 256:512], RQD(h), Isb[:, :], OP.mult, OP.add)
                        pS = PS()
                        for kc in range(2):
                            kb.mm(pS[:, kc * 256:(kc + 1) * 256], kd[:, c, kc * 128:(kc + 1) * 128], vt[:, c, :], r=True)
                        for kc in range(2):
                            kb.stt(S_r[:, h * 2 + kc, :], S_r[:, h * 2 + kc, :], float(gam ** 128),
                                   pS[:, kc * 256:(kc + 1) * 256], OP.mult, OP.add)
                        if main:
                            c_ = COL()
                            j = O256()
                            kb.act(j[:, :], o[:, :], AF.Identity, accum=c_[:, 0:1])
                            kb.act(j[:, :], o[:, :], AF.Square, accum=c_[:, 1:2])
                            kb.ts(c_[:, 2:3], c_[:, 0:1], 1.0 / 256.0, OP.mult)
                            kb.tt(c_[:, 3:4], c_[:, 2:3], c_[:, 2:3], OP.mult)
                            kb.stt(c_[:, 4:5], c_[:, 1:2], 1.0 / 256.0, c_[:, 3:4], OP.mult, OP.subtract)
                            kb.ts(c_[:, 4:5], c_[:, 4:5], EPS, OP.add)
                            kb.act(c_[:, 4:5], c_[:, 4:5], AF.Sqrt)
                            kb.recip(c_[:, 5:6], c_[:, 4:5])
                            kb.ts(o[:, :], o[:, :], c_[:, 2:3], OP.subtract, c_[:, 5:6], OP.mult)
                            kb.tt(o[:, :], o[:, :], rng[:, h * 256:(h + 1) * 256], OP.mult)
                            kb.tt(o[:, :], o[:, :], rnb[:, h * 256:(h + 1) * 256], OP.add, eng="pool")
                            kb.tt(o[:, :], o[:, :], gt_[:, c, :], OP.mult)
                            pm = PS()
                            for kc in range(2):
                                kb.tr(pm[:, kc * 128:(kc + 1) * 128], o[:, kc * 128:(kc + 1) * 128], ident)
                            kb.act(mixedT[:, 8 + h * 2: 10 + h * 2, cs], r3(pm[:, 0:256], 2), AF.Identity)

                CHK('ret')
                if blk == NBLK - 1:
                    kb.ts(S_g[:, :, :], S_g[:, :, :], flag[:, 0:1], OP.mult)
                    kb.ts(S_r[:, :, :], S_r[:, :, :], flag[:, 0:1], OP.mult)
                    kb.ts(carry[:, :, :], carry[:, :, :], flag[:, 0:1], OP.mult)
                if not main:
                    continue
                bi = t0 // TG
                h2T = hT
                for tt in range(NCH):
                    x_ = xt[tt % 2]
                    r0 = t0 + tt * 128
                    kb.dma("sp", x_[:, :], DV(xm[r0: r0 + 128, :]))
                    for dg in range(8):
                        wo = load_w(w_out, dg * 256, 256, WB1())
                        p = PS()
                        for k in range(16):
                            kb.mm(p[:, 0:256], mixedT[:, k, tt * 128:(tt + 1) * 128], wo[:, k, :], start=(k == 0), stop=(k == 15))
                        dsl = slice(dg * 256, (dg + 1) * 256)
                        tmp_ = FM()
                        kb.tt(tmp_[:, :], p[:, 0:256], gt1row[:, dsl], OP.mult)
                        kb.tt(x_[:, dsl], x_[:, dsl], tmp_[:, :], OP.add, eng="pool")
                    kb.dma("sp", View(x2s[bi], x2s_ap[r0: r0 + 128, :]), x_[:, :])
                    norm_mod_T(x_, 32, 48, h2T, tt, f32T=h2f)
                    p = PS()
                    for k in range(16):
                        kb.mm(p[:, 0:32], h2f[:, k, :], wr[:, k, :], start=(k == 0), stop=(k == 15))
                    lg = small
                    kb.tt(lg[:, 0:32], p[:, 0:32], br[:, :], OP.add)
                    c_ = COL()
                    cv_, lv_, lm_ = c_[:, 0:8], lg[:, 0:32], lg[:, 32:64]
                    kb.op("dve", lambda: nc.vector.max(out=cv_.ap, in_=lv_.ap), [lg], [c_])
                    kb.ts(lg[:, 32:64], lg[:, 0:32], c_[:, 3:4], OP.is_ge)
                    kb.ts(lg[:, 0:32], lg[:, 0:32], c_[:, 0:1], OP.subtract)
                    kb.act(lg[:, 0:32], lg[:, 0:32], AF.Exp)
                    kb.tt(lg[:, 0:32], lg[:, 0:32], lg[:, 32:64], OP.mult)
                    c2 = COL()
                    c2v = c2[:, 0:1]
                    kb.op("dve", lambda: nc.vector.tensor_reduce(out=c2v.ap, in_=lv_.ap,
                                                                 axis=mybir.AxisListType.X, op=OP.add), [lg], [c2])
                    kb.recip(c2[:, 1:2], c2[:, 0:1])
                    kb.ts(lg[:, 0:32], lg[:, 0:32], c2[:, 1:2], OP.mult)
                    kb.dma("sp", View(gts[bi], gts_ap[r0: r0 + 128, :]), lg[:, 0:32])
                kb.dma("sp", View(h2s[bi], h2s_ap.rearrange("p (k t) -> p k t", k=16)[:, :, t0:t0 + TB]), h2T[:, :, :])
            kb.barrier()

        with contextlib.ExitStack() as st2:
            fng = kb.sb("fng", [128, D], F32, st2)
            kb.dma("sp", fng[:, :], DV(fng_d[:, :]))
            b1t = kb.sb("b1t", [128, E, 32], F32, st2)
            kb.dma("sp", b1t[:, :, :], DV(b1_d[:, :].rearrange("p (e f) -> p e f", f=32)))
            kb.ts(b1t[:, :, 16:32], b1t[:, :, 16:32], 1.0, OP.add)
            b2t = kb.sb("b2t", [E, D], F32, st2)
            kb.dma("sp", b2t[:, :], DV(b2_d[:, :]))
            h2 = kb.sb("h2", [128, 16, TG], BF16, st2)
            gate = kb.sb("gate", [128, 4, E], F32, st2)
            gateT = kb.sb("gateT", [E, TG], F32, st2)
            acc = [kb.sb("acc%d" % i, [128, D], F32, st2) for i in range(4)]
            actT = [kb.sb("actT%d" % i, [128, 16, TG], BF16, st2) for i in range(1)]
            NEW = 6
            ew = [kb.sb("ew%d" % i, [128, TG], F32, st2) for i in range(NEW)]
            ewi = [0]

            def EW():
                ewi[0] = (ewi[0] + 1) % NEW
                return ew[ewi[0]]
            wb2 = [kb.sb("wb2_%d" % i, [128, 16, 512], BF16, st2) for i in range(3)]
            wb2i = [0]

            def WB2():
                wb2i[0] = (wb2i[0] + 1) % 3
                return wb2[wb2i[0]]
            x2t = [kb.sb("x2t%d" % i, [128, D], F32, st2) for i in range(1)]
            colm = [kb.sb("colm%d" % i, [128, 4], F32, st2) for i in range(4)]
            for tg in range(NTG if do_moe else 0):
                t0 = tg * TG
                kb.dma("sp", h2[:, :, :], View(h2s[tg], h2s_ap.rearrange("p (k t) -> p k t", k=16)[:, :, t0:t0 + TG]))
                kb.dma("sp", gate[:, :, :], View(gts[tg], gts_ap[t0:t0 + TG, :].rearrange("(c p) e -> p c e", p=128)))
                p = PS()
                for tt in range(4):
                    kb.tr(p[0:E, tt * 128:(tt + 1) * 128], gate[:, tt, :], ident)
                kb.copy(gateT[:, :], p[0:E, :])
                for tt in range(4):
                    for dg in range(4):
                        p = PS()
                        kb.mm(p[:, :], gateT[:, tt * 128:(tt + 1) * 128], b2t[:, dg * 512:(dg + 1) * 512])
                        kb.copy(acc[tt][:, dg * 512:(dg + 1) * 512], p[:, :], eng="act")
                for e in range(n_exp):
                    kb.maybe_roll()
                    aT = actT[0]
                    for g in range(4):
                        wg_ = load_w(w1[e], g * 512, 512, WB2())
                        wl_ = load_w(w1[e], D + g * 512, 512, WB2())
                        for j in range(4):
                            fj = g * 4 + j
                            pg = PS()
                            pl = PS()
                            for k in range(16):
                                kb.mm(pg[:, :], wg_[:, k, j * 128:(j + 1) * 128], h2[:, k, :], start=(k == 0), stop=(k == 15))
                            for k in range(16):
                                kb.mm(pl[:, :], wl_[:, k, j * 128:(j + 1) * 128], h2[:, k, :], start=(k == 0), stop=(k == 15))
                            gl, sg, tl = EW(), EW(), EW()
                            kb.ts(gl[:, :], pg[:, :], b1t[:, e, fj:fj + 1], OP.add, 7.0, OP.min)
                            kb.act(sg[:, :], gl[:, :], AF.Sigmoid, scale=1.702)
                            kb.ts(tl[:, :], pl[:, :], b1t[:, e, 16 + fj:17 + fj], OP.add, -6.0, OP.max)
                            kb.tt(sg[:, :], sg[:, :], gl[:, :], OP.mult, eng="pool")
                            kb.stt(aT[:, fj, :], tl[:, :], 8.0, sg[:, :], OP.min, OP.mult)
                    for dg in range(4):
                        w2_ = load_w(w2[e], dg * 512, 512, WB2())
                        for tt in range(4):
                            p = PS()
                            for k in range(16):
                                kb.mm(p[:, :], aT[:, k, tt * 128:(tt + 1) * 128], w2_[:, k, :], start=(k == 0), stop=(k == 15))
                            dsl = slice(dg * 512, (dg + 1) * 512)
                            kb.stt(acc[tt][:, dsl], p[:, :], gate[:, tt, e:e + 1], acc[tt][:, dsl], OP.mult, OP.add)
                for tt in range(4):
                    x_ = x2t[0]
                    r0 = t0 + tt * 128
                    kb.dma("sp", x_[:, :], View(x2s[tg], x2s_ap[r0: r0 + 128, :]))
                    kb.tt(acc[tt][:, :], acc[tt][:, :], gt2row[:, :], OP.mult)
                    kb.tt(x_[:, :], x_[:, :], acc[tt][:, :], OP.add, eng="pool")
                    c_ = colm[tt]
                    kb.act(acc[tt][:, :], x_[:, :], AF.Square, accum=c_[:, 0:1])
                    kb.ts(c_[:, 1:2], c_[:, 0:1], 1.0 / D, OP.mult, EPS, OP.add)
                    kb.act(c_[:, 1:2], c_[:, 1:2], AF.Sqrt)
                    kb.recip(c_[:, 1:2], c_[:, 1:2])
                    kb.stt(x_[:, :], x_[:, :], c_[:, 1:2], fng[:, :], OP.mult, OP.mult)
                    kb.dma("sp", DV(out_d[r0: r0 + 128, :]), x_[:, :])
            kb.barrier()
      except _Stop:
        kb.barrier()
    return nc


h2f_keep = [None]


def make_consts():
    c = np.zeros((128, 1536), np.float32)
    i = np.arange(128)
    c[:, 0:128] = np.eye(128)
    c[:, 128:256] = (i[:, None] <= i[None, :])
    c[:, 256:384] = (i[None, :] < i[:, None])
    c[:, 384:512] = (i[None, :] <= i[:, None])
    c[:, 512:640] = 1.0
    for h in range(4):
        lg = np.log1p(-np.exp2(-5.0 - h))
        d = (i[None, :] - i[:, None]).astype(np.float64)
        m = np.where(d >= 0, np.exp(d * lg), 0.0)
        c[:, 640 + h * 128: 640 + (h + 1) * 128] = m
        c[:, 1152 + h] = np.exp((i + 1.0) * lg)
        c[:, 1156 + h] = np.exp((127.0 - i) * lg)
    c[:, 1160] = np.power(np.float32(10000.0), -np.linspace(0.0, 1.0, 128, dtype=np.float32)).astype(np.float32)
    return c


_NC = [None]


def kernel(x, c, positions, ada_w, ada_b, norm1_g, w_in, conv_w, gdn_a_log, gdn_dt_bias, gdn_norm_g,
           ret_norm_g, ret_norm_b, w_out, norm2_g, w_router, b_router, w1, b1, w2, b2, final_norm_g):
    f = np.float32
    x = np.asarray(x, f)

    def rep(v, n=128):
        v = np.asarray(v, f).reshape(1, -1)
        return np.ascontiguousarray(np.broadcast_to(v, (n, v.shape[1])))

    def pk(v):
        return np.ascontiguousarray(np.asarray(v, f).reshape(-1, 128).T)
    shared = {
        "ada_w": np.ascontiguousarray(np.asarray(ada_w, f)[0]),
        "ada_b": rep(ada_b[0]),
        "n1g": rep(norm1_g[0]), "n2g": rep(norm2_g[0]), "fng": rep(final_norm_g),
        "w_in": np.ascontiguousarray(np.asarray(w_in, f)[0]),
        "convw": np.ascontiguousarray(np.asarray(conv_w, f)[0].T.reshape(24, 128, 4).transpose(1, 0, 2).reshape(128, 96)),
        "alog": rep(gdn_a_log[0]), "dtb": rep(gdn_dt_bias[0]), "gng": rep(gdn_norm_g[0]),
        "rng": rep(ret_norm_g[0]), "rnb": rep(ret_norm_b[0]),
        "w_out": np.ascontiguousarray(np.asarray(w_out, f)[0]),
        "wr": np.ascontiguousarray(np.asarray(w_router, f)[0].reshape(16, 128, 32).transpose(1, 0, 2).reshape(128, 512)),
        "br": rep(b_router[0]),
        "w1": np.ascontiguousarray(np.asarray(w1, f)[0]),
        "b1": np.ascontiguousarray(np.asarray(b1, f)[0].reshape(E, 32, 128).transpose(2, 0, 1).reshape(128, E * 32)),
        "w2": np.ascontiguousarray(np.asarray(w2, f)[0]),
        "b2": np.ascontiguousarray(np.asarray(b2, f)[0]),
        "consts": make_consts(),
    }
    pos = np.asarray(positions, np.int32)
    in_maps = []
    for core in range(8):
        b, half = core // 2, core % 2
        m = dict(shared)
        m["xm"] = np.ascontiguousarray(x[b, half * NTOK:(half + 1) * NTOK])
        m["xp"] = np.ascontiguousarray(x[b, 0:NTOK])
        m["posm"] = np.ascontiguousarray(np.broadcast_to(pos[b, half * NTOK:(half + 1) * NTOK][None, :], (128, NTOK)))
        m["posp"] = np.ascontiguousarray(np.broadcast_to(pos[b, 0:NTOK][None, :], (128, NTOK)))
        m["flag"] = np.full((128, 1), float(half), f)
        m["c"] = pk(np.asarray(c, f)[b])
        in_maps.append(m)
    if _NC[0] is None:
        _NC[0] = build_program()
    res = run_bass_kernel_spmd(_NC[0], in_maps, core_ids=list(range(8)))
    out = np.zeros((4, 2 * NTOK, D), f)
    for core in range(8):
        b, half = core // 2, core % 2
        out[b, half * NTOK:(half + 1) * NTOK] = res.results[core]["out"]
    return out
```

```python
import contextlib
import numpy as np
import concourse.bass as bass
import concourse.mybir as mybir
from concourse.bass_utils import run_bass_kernel_spmd

F32 = mybir.dt.float32
BF16 = mybir.dt.bfloat16
I32 = mybir.dt.int32
AF = mybir.ActivationFunctionType
OP = mybir.AluOpType

D = 2048
NTOK = 2048
TB = 256
NBLK = 8
TG = 512
NTG = 4
NCH = TB // 128
E = 32
EPS = 1e-6
IN_COLS = 8208
SEM_LIM = 20000
MATH_PI = float(np.pi)


class View:
    __slots__ = ("t", "ap")

    def __init__(self, t, ap):
        self.t = t
        self.ap = ap


class T:
    def __init__(self, h, excl=False):
        self.h = h
        self.lastw = None
        self.reads = {}
        self.excl = excl

    def __getitem__(self, idx):
        return View(self, self.h[idx])


class KB:
    def __init__(self, nc):
        self.nc = nc
        self.es = contextlib.ExitStack()
        self.eng = {"pe": nc.tensor, "act": nc.scalar, "dve": nc.vector, "pool": nc.gpsimd, "sp": nc.sync}
        self.cur = {}
        self.seen = {k: {} for k in self.eng}
        self.nsem = 0
        self.last_ev = {}
        self.dma_pool = {}
        self.dma_idx = {}
        self.all_dma = {}
        self.dead = False
        for k in ("pe", "act", "dve", "pool"):
            self._new_epoch(k)
        for q in ("sp", "pool"):
            self.dma_pool[q] = [[self._sem("dq_%s%d" % (q, i)), 0] for i in range(12)]
            self.dma_idx[q] = 0

    def _sem(self, name):
        self.nsem += 1
        h = self.es.enter_context(self.nc.semaphore("%s_%d" % (name, self.nsem)))
        return (self.nsem, h)

    def _new_epoch(self, e):
        self.cur[e] = [self._sem("e_" + e), 0]

    def sb(self, name, shape, dt, stack=None):
        return T((stack or self.es).enter_context(self.nc.sbuf_tensor("s_" + name, shape, dt)))

    def _wait(self, e, need):
        seen = self.seen[e]
        own = self.cur[e][0][0] if e in self.cur else None
        for (key, h, v) in need:
            if e == "pe" and key == own:
                continue
            if seen.get(key, 0) >= v:
                continue
            self.eng[e].wait_ge(h, v)
            seen[key] = v

    def _deps(self, R, W):
        need = []
        for t in R:
            if t is not None and t.lastw is not None:
                need.append(t.lastw)
            if t is not None and t.excl:
                need.extend(t.reads.values())
        for t in W:
            if t is None:
                continue
            if t.lastw is not None:
                need.append(t.lastw)
            need.extend(t.reads.values())
        return need

    def _record(self, ev, R, W):
        for t in R:
            if t is not None:
                t.reads[ev[0]] = ev
        for t in W:
            if t is not None:
                t.lastw = ev
                t.reads = {}

    def op(self, e, fn, R, W):
        if self.dead:
            return
        self._wait(e, self._deps(R, W))
        inst = fn()
        c = self.cur[e]
        c[1] += 1
        inst.then_inc(c[0][1], 1)
        ev = (c[0][0], c[0][1], c[1])
        self.last_ev[c[0][0]] = (e, ev)
        if c[1] >= 30000:
            self._new_epoch(e)
        self._record(ev, R, W)

    def dma(self, q, out, in_, **kw):
        if self.dead:
            return None
        R = [in_.t]
        W = [out.t]
        pool = self.dma_pool[q]
        i = self.dma_idx[q]
        self.dma_idx[q] = (i + 1) % len(pool)
        slot = pool[i]
        need = self._deps(R, W)
        if slot[1] > 0:
            need.append((slot[0][0], slot[0][1], slot[1]))
        self._wait(q, need)
        inst = self.eng[q].dma_start(out=out.ap, in_=in_.ap, **kw)
        slot[1] += 16
        if slot[1] >= SEM_LIM:
            pool[i] = [self._sem("dq_%s" % q), 0]
            slot2 = pool[i]
            slot2[1] = 16
            inst.then_inc(slot2[0][1], 16)
            ev = (slot2[0][0], slot2[0][1], 16)
        else:
            inst.then_inc(slot[0][1], 16)
            ev = (slot[0][0], slot[0][1], slot[1])
        self.all_dma[ev[0]] = ev
        self._record(ev, R, W)
        return ev

    def maybe_roll(self, lim=8000):
        if self.dead:
            return
        for e in list(self.cur):
            if self.cur[e][1] >= lim:
                self._new_epoch(e)

    def barrier(self):
        if self.dead:
            return
        evs = [ev for (_, ev) in self.last_ev.values()] + list(self.all_dma.values())
        for e in self.eng:
            self._wait_all(e, evs)

    def _wait_all(self, e, evs):
        seen = self.seen[e]
        for (key, h, v) in evs:
            if seen.get(key, 0) >= v:
                continue
            self.eng[e].wait_ge(h, v)
            seen[key] = v

    def mm(self, out, lhsT, rhs, start=True, stop=True):
        self.op("pe", lambda: self.nc.tensor.matmul(out.ap, lhsT=lhsT.ap, rhs=rhs.ap, start=start, stop=stop),
                [lhsT.t, rhs.t], [out.t])

    def tr(self, out, in_, ident):
        self.op("pe", lambda: self.nc.tensor.transpose(out.ap, in_.ap, ident.ap), [in_.t, ident.t], [out.t])

    def act(self, out, in_, func, bias=None, scale=None, accum=None):
        R = [in_.t]
        W = [out.t]
        kw = {}
        if bias is not None:
            if isinstance(bias, View):
                R.append(bias.t)
                kw["bias"] = bias.ap
            else:
                kw["bias"] = float(bias)
        if scale is not None:
            if isinstance(scale, View):
                R.append(scale.t)
                kw["scale"] = scale.ap
            else:
                kw["scale"] = float(scale)
        if accum is not None:
            W.append(accum.t)
            kw["accum_out"] = accum.ap
        self.op("act", lambda: self.nc.scalar.activation(out=out.ap, in_=in_.ap, func=func, **kw), R, W)

    def ts(self, out, in0, s1, op0, s2=None, op1=None, eng="dve"):
        R = [in0.t]

        def cv(s):
            if isinstance(s, View):
                R.append(s.t)
                return s.ap
            return None if s is None else float(s)
        a1 = cv(s1)
        a2 = cv(s2)
        kw = {}
        if op1 is not None:
            kw["op1"] = op1
        E_ = self.eng[eng]
        self.op(eng, lambda: E_.tensor_scalar(out=out.ap, in0=in0.ap, scalar1=a1, scalar2=a2, op0=op0, **kw),
                R, [out.t])

    def tt(self, out, in0, in1, op, eng="dve"):
        E_ = self.eng[eng]
        self.op(eng, lambda: E_.tensor_tensor(out=out.ap, in0=in0.ap, in1=in1.ap, op=op),
                [in0.t, in1.t], [out.t])

    def stt(self, out, in0, scalar, in1, op0, op1):
        R = [in0.t, in1.t]
        if isinstance(scalar, View):
            R.append(scalar.t)
            s = scalar.ap
        else:
            s = float(scalar)
        self.op("dve", lambda: self.nc.vector.scalar_tensor_tensor(out=out.ap, in0=in0.ap, scalar=s, in1=in1.ap,
                                                                   op0=op0, op1=op1), R, [out.t])

    def copy(self, out, in_, eng="dve"):
        if eng == "act":
            self.act(out, in_, AF.Identity)
        else:
            E_ = self.eng[eng]
            self.op(eng, lambda: E_.tensor_copy(out=out.ap, in_=in_.ap), [in_.t], [out.t])

    def memset(self, out, val, eng="dve"):
        E_ = self.eng[eng]
        self.op(eng, lambda: E_.memset(out.ap, val), [], [out.t])

    def recip(self, out, in_):
        self.op("dve", lambda: self.nc.vector.reciprocal(out=out.ap, in_=in_.ap), [in_.t], [out.t])


class _Stop(Exception):
    pass


def build_program(n_exp=E, dbg=False, do_moe=True, stop=None):
    nc = bass.Bass("TRN2", target_bir_lowering=False)
    kb = KB(nc)

    def dram_in(name, shape, dt=F32):
        return nc.dram_tensor(name, shape, dt, kind="ExternalInput").ap()

    xm = dram_in("xm", [NTOK, D])
    xp = dram_in("xp", [NTOK, D])
    posm = dram_in("posm", [128, NTOK], I32)
    posp = dram_in("posp", [128, NTOK], I32)
    flag_d = dram_in("flag", [128, 1])
    c_d = dram_in("c", [128, 16])
    ada_w = dram_in("ada_w", [D, 6 * D])
    ada_b_d = dram_in("ada_b", [128, 6 * D])
    n1g_d = dram_in("n1g", [128, D])
    n2g_d = dram_in("n2g", [128, D])
    fng_d = dram_in("fng", [128, D])
    w_in = dram_in("w_in", [D, IN_COLS])
    convw_d = dram_in("convw", [128, 24 * 4])
    alog_d = dram_in("alog", [128, 8])
    dtb_d = dram_in("dtb", [128, 8])
    gng_d = dram_in("gng", [128, 128])
    rng_d = dram_in("rng", [128, 1024])
    rnb_d = dram_in("rnb", [128, 1024])
    w_out = dram_in("w_out", [D, D])
    wr_d = dram_in("wr", [128, 16 * 32])
    br_d = dram_in("br", [128, 32])
    w1 = dram_in("w1", [n_exp, D, 2 * D])
    b1_d = dram_in("b1", [128, E * 32])
    w2 = dram_in("w2", [n_exp, D, D])
    b2_d = dram_in("b2", [E, D])
    consts_d = dram_in("consts", [128, 1536])
    out_d = nc.dram_tensor("out", [NTOK, D], F32, kind="ExternalOutput").ap()
    ik = "ExternalOutput" if dbg else "Internal"
    x2s_ap = nc.dram_tensor("x2s", [NTOK, D], F32, kind=ik).ap()
    h2s_ap = nc.dram_tensor("h2s", [128, 16 * NTOK], BF16, kind=ik).ap()
    gts_ap = nc.dram_tensor("gts", [NTOK, E], F32, kind=ik).ap()
    x2s = [T(None) for _ in range(NTG)]
    h2s = [T(None) for _ in range(NTG)]
    gts = [T(None) for _ in range(NTG)]

    def DV(ap):
        return View(None, ap)

    def r3(v, j):
        return View(v.t, v.ap.rearrange("p (j t) -> p j t", j=j))

    def CHK(tag):
        if stop == tag and not kb.dead:
            kb.barrier()
            kb.dead = True

    with kb.es:
      try:
        cst = kb.sb("cst", [128, 1536], F32)
        ident = cst[:, 0:128]
        TRI = cst[:, 128:256]
        MLS = cst[:, 256:384]
        MLI = cst[:, 384:512]
        ONES = cst[:, 512:640]

        def RDT(h):
            return cst[:, 640 + h * 128: 640 + (h + 1) * 128]

        def RQD(h):
            return cst[:, 1152 + h: 1153 + h]

        def RKD(h):
            return cst[:, 1156 + h: 1157 + h]
        INVF = cst[:, 1160:1161]
        kb.dma("sp", cst[:, :], DV(consts_d[:, :]))
        flag = kb.sb("flag", [128, 1], F32)
        kb.dma("sp", flag[:, :], DV(flag_d[:, :]))
        gt1row = kb.sb("gt1row", [128, D], F32)
        gt2row = kb.sb("gt2row", [128, D], F32)
        modc = kb.sb("modc", [128, 64], F32)
        ps = [T(kb.es.enter_context(nc.psum_tensor("ps%d" % i, [128, 512], F32)), excl=True) for i in range(8)]
        psi = [0]

        def PS():
            psi[0] = (psi[0] + 1) % 8
            return ps[psi[0]]

        def load_w(src2d, c0, ncols, dst):
            kb.dma("pool", dst[:, :, 0:ncols], DV(src2d[:, c0:c0 + ncols].rearrange("(k p) c -> p k c", p=128)))
            return dst

        with contextlib.ExitStack() as st0:
            cond = kb.sb("cond", [128, 16], F32, st0)
            kb.dma("sp", cond[:, :], DV(c_d[:, :]))
            kb.act(cond[:, :], cond[:, :], AF.Silu)
            condb = kb.sb("condb", [128, 16, 128], F32, st0)
            for k in range(16):
                kb.ts(condb[:, k, :], ONES, cond[:, k:k + 1], OP.mult)
            adab = kb.sb("adab", [128, 6 * D], F32, st0)
            kb.dma("sp", adab[:, :], DV(ada_b_d[:, :]))
            aw = [kb.sb("aw%d" % i, [128, 16, 512], F32, st0) for i in range(2)]
            modr = kb.sb("modr", [128, 6, D], F32, st0)
            for g in range(24):
                a = aw[g % 2]
                kb.dma("sp", a[:, :, :], DV(ada_w[:, g * 512:(g + 1) * 512].rearrange("(k p) c -> p k c", p=128)))
                p = PS()
                for k in range(16):
                    kb.mm(p[:, :], condb[:, k, :], a[:, k, :], start=(k == 0), stop=(k == 15))
                kb.tt(modr[:, g // 4, (g % 4) * 512:(g % 4 + 1) * 512], p[:, :], adab[:, g * 512:(g + 1) * 512], OP.add)
            tmpg = kb.sb("tmpg", [128, D], F32, st0)
            for (src, gd) in ((1, n1g_d), (4, n2g_d)):
                kb.dma("sp", tmpg[:, :], DV(gd[:, :]))
                kb.stt(modr[:, src, :], modr[:, src, :], 1.0, tmpg[:, :], OP.add, OP.mult)
            kb.copy(gt1row[:, :], modr[:, 2, :])
            kb.copy(gt2row[:, :], modr[:, 5, :], eng="pool")
            for vi, src in enumerate((1, 0, 4, 3)):
                for g in range(4):
                    p = PS()
                    for j in range(4):
                        kc = g * 4 + j
                        kb.tr(p[:, j * 128:(j + 1) * 128], modr[:, src, kc * 128:(kc + 1) * 128], ident)
                    for j in range(4):
                        kc = g * 4 + j
                        kb.copy(modc[:, vi * 16 + kc: vi * 16 + kc + 1], p[:, j * 128: j * 128 + 1])
            kb.barrier()
        CHK('p0')

        with contextlib.ExitStack() as st1:
            S_g = kb.sb("S_g", [128, 8, 128], F32, st1)
            S_r = kb.sb("S_r", [128, 8, 256], F32, st1)
            carry = kb.sb("carry", [128, 24, 4], F32, st1)
            kb.memset(S_g[:, :, :], 0.0)
            kb.memset(S_r[:, :, :], 0.0)
            kb.memset(carry[:, :, :], 0.0)
            convw = kb.sb("convw", [128, 24, 4], F32, st1)
            kb.dma("sp", convw[:, :, :], DV(convw_d[:, :].rearrange("p (c i) -> p c i", i=4)))
            negA = kb.sb("negA", [128, 8], F32, st1)
            dtb = kb.sb("dtb", [128, 8], F32, st1)
            kb.dma("sp", negA[:, :], DV(alog_d[:, :]))
            kb.dma("sp", dtb[:, :], DV(dtb_d[:, :]))
            kb.act(negA[:, :], negA[:, :], AF.Exp)
            kb.ts(negA[:, :], negA[:, :], -1.0, OP.mult)
            gng = kb.sb("gng", [128, 128], F32, st1)
            kb.dma("sp", gng[:, :], DV(gng_d[:, :]))
            rng = kb.sb("rng", [128, 1024], F32, st1)
            rnb = kb.sb("rnb", [128, 1024], F32, st1)
            kb.dma("sp", rng[:, :], DV(rng_d[:, :]))
            kb.dma("sp", rnb[:, :], DV(rnb_d[:, :]))
            wr = kb.sb("wr", [128, 16, 32], F32, st1)
            kb.dma("sp", wr[:, :, :], DV(wr_d[:, :].rearrange("p (k e) -> p k e", e=32)))
            br = kb.sb("br", [128, 32], F32, st1)
            kb.dma("sp", br[:, :], DV(br_d[:, :]))

            xt = [kb.sb("xt%d" % i, [128, D], F32, st1) for i in range(2)]
            hT = kb.sb("hT", [128, 16, TB], BF16, st1)
            mixedT = kb.sb("mixedT", [128, 16, TB], BF16, st1)
            h2f = kb.sb("h2f", [128, 16, 128], F32, st1)
            small = kb.sb("small", [128, 64], F32, st1)
            ba = kb.sb("ba", [128, NCH, 16], F32, st1)
            beta = kb.sb("beta", [128, NCH, 8], F32, st1)
            gs_ = kb.sb("gs", [128, NCH, 8], F32, st1)
            gcol = kb.sb("gcol", [128, NCH, 8], F32, st1)
            eg = kb.sb("eg", [128, NCH, 8], F32, st1)
            nbeg = kb.sb("nbeg", [128, NCH, 8], F32, st1)
            kes = kb.sb("kes", [128, NCH, 8], F32, st1)
            cdt = kb.sb("cd", [128, NCH, 8], F32, st1)
            tmp8 = kb.sb("tmp8", [128, NCH, 8], F32, st1)
            tmp8b = kb.sb("tmp8b", [128, NCH, 8], F32, st1)
            cosT = kb.sb("cosT", [128, TB], F32, st1)
            sinT = kb.sb("sinT", [128, TB], F32, st1)
            posi = kb.sb("posi", [128, TB], I32, st1)
            uw = [kb.sb("uw%d" % i, [128, TB + 3], F32, st1) for i in range(2)]
            NFM = 12
            fm = [kb.sb("fm%d" % i, [128, TB], F32, st1) for i in range(NFM)]
            fmi = [0]

            def FM():
                fmi[0] = (fmi[0] + 1) % NFM
                return fm[fmi[0]]
            slot_q = [kb.sb("slq%d" % i, [128, TB], F32, st1) for i in range(4)]
            slot_k = [kb.sb("slk%d" % i, [128, TB], F32, st1) for i in range(4)]
            slot_kt = [kb.sb("slkt%d" % i, [128, NCH, 128], F32, st1) for i in range(4)]
            slot_vt = [kb.sb("slvt%d" % i, [128, NCH, 128], F32, st1) for i in range(4)]
            slot_z = [kb.sb("slz%d" % i, [128, NCH, 128], F32, st1) for i in range(4)]
            slot_aq = [kb.sb("slaq%d" % i, [128, 128], F32, st1) for i in range(4)]
            slot_sq = [[kb.sb("slsq%d_%d" % (i, j), [128, 128], F32, st1) for j in range(10)] for i in range(4)]
            NSQ = 4
            sq = [kb.sb("sq%d" % i, [128, 128], F32, st1) for i in range(NSQ)]
            sqi = [0]

            def SQ():
                sqi[0] = (sqi[0] + 1) % NSQ
                return sq[sqi[0]]
            tk = [kb.sb("tk%d" % i, [128, NCH, 256], F32, st1) for i in range(4)]
            wq = [kb.sb("wq%d" % i, [128, 16, 128], BF16, st1) for i in range(3)]
            wqi = [0]

            def WQ():
                wqi[0] = (wqi[0] + 1) % 3
                return wq[wqi[0]]
            wb1 = [kb.sb("wb1_%d" % i, [128, 16, 256], BF16, st1) for i in range(2)]
            wb1i = [0]

            def WB1():
                wb1i[0] = (wb1i[0] + 1) % 2
                return wb1[wb1i[0]]
            col = [kb.sb("col%d" % i, [128, 8], F32, st1) for i in range(8)]
            coli = [0]

            def COL():
                coli[0] = (coli[0] + 1) % 8
                return col[coli[0]]
            o256 = [kb.sb("o256_%d" % i, [128, 256], F32, st1) for i in range(4)]
            o2i = [0]

            def O256():
                o2i[0] = (o2i[0] + 1) % 4
                return o256[o2i[0]]

            def rstd_from_ss(ss, n, dst):
                kb.ts(dst, ss, 1.0 / n, OP.mult, EPS, OP.add)
                kb.act(dst, dst, AF.Sqrt)
                kb.recip(dst, dst)

            def norm_mod_T(xtile, gbase, sbase, dstT, tt, f32T=None):
                c_ = COL()
                j_ = FM()
                for q4 in range(D // TB):
                    kb.act(j_[:, :], xtile[:, q4 * TB:(q4 + 1) * TB], AF.Square, accum=c_[:, 2 + (q4 % 4):3 + (q4 % 4)]) \
                        if False else None
                kb.act(xtile_junk[:, :], xtile[:, :], AF.Square, accum=c_[:, 0:1])
                rstd_from_ss(c_[:, 0:1], float(D), c_[:, 1:2])
                kb.ts(xtile[:, :], xtile[:, :], c_[:, 1:2], OP.mult)
                for g in range(4):
                    p = PS()
                    for j in range(4):
                        kc = g * 4 + j
                        kb.tr(p[:, j * 128:(j + 1) * 128], xtile[:, kc * 128:(kc + 1) * 128], ident)
                    for j in range(4):
                        kc = g * 4 + j
                        kb.act(dstT[:, kc, tt * 128:(tt + 1) * 128], p[:, j * 128:(j + 1) * 128], AF.Identity,
                               scale=modc[:, gbase + kc: gbase + kc + 1], bias=modc[:, sbase + kc: sbase + kc + 1])
                        if f32T is not None:
                            kb.ts(f32T[:, kc, :], p[:, j * 128:(j + 1) * 128], modc[:, gbase + kc: gbase + kc + 1], OP.mult,
                                  modc[:, sbase + kc: sbase + kc + 1], OP.add)

            xtile_junk = kb.sb("xjunk", [128, D], BF16, st1)

            def proj_fm(wtile, dst_ps):
                for k in range(16):
                    kb.mm(dst_ps, wtile[:, k, 0:128], hT[:, k, :], start=(k == 0), stop=(k == 15))

            def proj_tm(wtile, ncols, tt, dst_ps):
                for k in range(16):
                    kb.mm(dst_ps, hT[:, k, tt * 128:(tt + 1) * 128], wtile[:, k, 0:ncols], start=(k == 0), stop=(k == 15))

            for blk in range(2 * NBLK):
                kb.maybe_roll()
                main = blk >= NBLK
                xsrc = xm if main else xp
                psrc = posm if main else posp
                t0 = (blk % NBLK) * TB
                for tt in range(NCH):
                    x_ = xt[tt % 2]
                    kb.dma("sp", x_[:, :], DV(xsrc[t0 + tt * 128: t0 + (tt + 1) * 128, :]))
                    norm_mod_T(x_, 0, 16, hT, tt)
                CHK('norm1')
                ang, ang2 = FM(), FM()
                kb.dma("sp", posi[:, :], DV(psrc[:, t0:t0 + TB]))
                kb.copy(ang[:, :], posi[:, :])
                kb.ts(ang[:, :], ang[:, :], INVF, OP.mult)
                for (shift, dst) in ((0.0, sinT), (MATH_PI / 2, cosT)):
                    kb.ts(ang2[:, :], ang[:, :], shift, OP.add, 1.0 / (2 * MATH_PI), OP.mult)
                    kb.copy(posi[:, :], ang2[:, :])
                    kb.copy(ang2[:, :], posi[:, :])
                    kb.stt(ang2[:, :], ang2[:, :], -2 * MATH_PI, ang[:, :], OP.mult, OP.add)
                    kb.ts(ang2[:, :], ang2[:, :], shift, OP.add)
                    m_ = FM()
                    kb.ts(m_[:, :], ang2[:, :], MATH_PI, OP.is_gt)
                    kb.stt(ang2[:, :], m_[:, :], -2 * MATH_PI, ang2[:, :], OP.mult, OP.add)
                    kb.ts(m_[:, :], ang2[:, :], -MATH_PI, OP.is_lt)
                    kb.stt(ang2[:, :], m_[:, :], 2 * MATH_PI, ang2[:, :], OP.mult, OP.add)
                    kb.ts(ang2[:, :], ang2[:, :], MATH_PI, OP.min, -MATH_PI, OP.max)
                    kb.act(dst[:, :], ang2[:, :], AF.Sin)
                CHK('rot')
                wba = WQ()
                kb.dma("pool", wba[:, :, 0:16], DV(w_in[:, 4096:4112].rearrange("(k p) c -> p k c", p=128)))
                for tt in range(NCH):
                    p = PS()
                    proj_tm(wba, 16, tt, p[:, 0:16])
                    kb.copy(ba[:, tt, :], p[:, 0:16])
                kb.act(beta[:, :, :], ba[:, :, 0:8], AF.Sigmoid)
                for tt in range(NCH):
                    kb.tt(tmp8[:, tt, :], ba[:, tt, 8:16], dtb[:, :], OP.add)
                kb.stt(tmp8b[:, :, :], tmp8[:, :, :], -1.0, tmp8[:, :, :], OP.mult, OP.max)
                kb.act(tmp8b[:, :, :], tmp8b[:, :, :], AF.Exp, scale=-1.0)
                kb.act(tmp8b[:, :, :], tmp8b[:, :, :], AF.Ln, bias=1.0)
                kb.stt(tmp8[:, :, :], tmp8[:, :, :], 0.0, tmp8b[:, :, :], OP.max, OP.add)
                for tt in range(NCH):
                    kb.tt(gs_[:, tt, :], tmp8[:, tt, :], negA[:, :], OP.mult)
                for tt in range(NCH):
                    p = PS()
                    kb.mm(p[:, 0:8], TRI, gs_[:, tt, :])
                    kb.mm(p[:, 8:16], ONES, gs_[:, tt, :])
                    kb.copy(gcol[:, tt, :], p[:, 0:8])
                    kb.act(eg[:, tt, :], p[:, 0:8], AF.Exp)
                    kb.act(cdt[:, tt, :], p[:, 8:16], AF.Exp)
                    kb.tt(tmp8b[:, tt, :], p[:, 8:16], gcol[:, tt, :], OP.subtract)
                    kb.act(kes[:, tt, :], tmp8b[:, tt, :], AF.Exp)
                kb.stt(nbeg[:, :, :], beta[:, :, :], -1.0, eg[:, :, :], OP.mult, OP.mult)

                CHK('ba')
                def gdn_pre(h, sl):
                    qT, kT = slot_q[sl], slot_k[sl]
                    vT = None
                    for which in range(3):
                        cc = which * 8 + h
                        wt_ = load_w(w_in, cc * 128, 128, WQ())
                        p = PS()
                        proj_fm(wt_, p[:, 0:TB])
                        u = uw[which % 2]
                        kb.copy(u[:, 0:3], carry[:, cc, 0:3], eng="pool")
                        kb.act(u[:, 3:TB + 3], p[:, 0:TB], AF.Identity)
                        y = (qT, kT, None)[which] or FM()
                        kb.ts(y[:, :], u[:, 0:TB], convw[:, cc, 0:1], OP.mult)
                        for i in range(1, 4):
                            kb.stt(y[:, :], u[:, i:TB + i], convw[:, cc, i:i + 1], y[:, :], OP.mult, OP.add)
                        kb.copy(carry[:, cc, 0:3], u[:, TB:TB + 3], eng="pool")
                        kb.act(y[:, :], y[:, :], AF.Silu)
                        if which == 2:
                            vT = y
                    for (src, extra) in ((qT, float(np.log(128.0 ** -0.5))), (kT, 0.0)):
                        s2 = FM()
                        kb.act(s2[:, :], src[:, :], AF.Square)
                        p = PS()
                        kb.mm(p[:, 0:TB], ONES, s2[:, :])
                        kb.act(s2[:, :], p[:, 0:TB], AF.Ln, bias=EPS)
                        kb.act(s2[:, :], s2[:, :], AF.Exp, scale=-0.5, bias=extra)
                        kb.tt(src[:, :], src[:, :], s2[:, :], OP.mult)
                    for (src, dst) in ((kT, slot_kt[sl]), (vT, slot_vt[sl])):
                        p = PS()
                        for c in range(NCH):
                            kb.tr(p[:, c * 128:(c + 1) * 128], src[:, c * 128:(c + 1) * 128], ident)
                        kb.act(dst[:, :, :], r3(p[:, 0:TB], NCH), AF.Identity)
                    if main:
                        wz = load_w(w_in, 3072 + h * 128, 128, WQ())
                        for tt in range(NCH):
                            p = PS()
                            proj_tm(wz, 128, tt, p[:, 0:128])
                            kb.act(slot_z[sl][:, tt, :], p[:, 0:128], AF.Silu)

                def gdn_chunks(h, sl):
                    qT, kT, ktok, vtok, zs = slot_q[sl], slot_k[sl], slot_kt[sl], slot_vt[sl], slot_z[sl]
                    pool_ = slot_sq[sl]
                    pi = [0]

                    def SQh():
                        pi[0] = (pi[0] + 1) % len(pool_)
                        return pool_[pi[0]]
                    AQKT = slot_aq[sl]
                    for c in range(NCH):
                        cs = slice(c * 128, (c + 1) * 128)
                        bcol = beta[:, c, h:h + 1]
                        GSB = SQh()
                        kb.ts(GSB[:, :], ONES, gs_[:, c, h:h + 1], OP.mult, 0.0, OP.add, eng="pool")
                        yield
                        pRB = PS()
                        kb.mm(pRB[:, 0:128], GSB[:, :], TRI)
                        NE = SQh()
                        kb.ts(NE[:, :], pRB[:, 0:128], gcol[:, c, h:h + 1], OP.subtract, 0.0, OP.max)
                        EX = SQh()
                        kb.act(EX[:, :], NE[:, :], AF.Exp, scale=-1.0)
                        yield
                        ML = SQh()
                        kb.stt(ML[:, :], EX[:, :], bcol, MLS, OP.mult, OP.mult)
                        EXI = SQh()
                        kb.tt(EXI[:, :], EX[:, :], MLI, OP.mult, eng="pool")
                        yield
                        pG = PS()
                        kb.mm(pG[:, 0:128], kT[:, cs], kT[:, cs])
                        kb.mm(pG[:, 128:256], qT[:, cs], kT[:, cs])
                        L = SQh()
                        kb.tt(L[:, :], pG[:, 0:128], ML[:, :], OP.mult)
                        AQK = SQh()
                        kb.tt(AQK[:, :], pG[:, 128:256], EXI[:, :], OP.mult)
                        yield
                        pT = PS()
                        kb.tr(pT[:, 0:128], L[:, :], ident)
                        kb.tr(pT[:, 128:256], AQK[:, :], ident)
                        U = SQh()
                        kb.copy(U[:, :], pT[:, 0:128], eng="act")
                        kb.copy(AQKT[:, :], pT[:, 128:256], eng="act")
                        yield
                        P = SQh()
                        kb.tt(P[:, :], ident, U[:, :], OP.subtract)
                        Lp, Up = L, U
                        for k in range(1, 7):
                            pp = PS()
                            kb.mm(pp[:, 0:128], Up[:, :], Lp[:, :])
                            if k < 6:
                                kb.mm(pp[:, 128:256], Lp[:, :], Up[:, :])
                            Ln = SQh()
                            kb.copy(Ln[:, :], pp[:, 0:128], eng="act")
                            if k < 6:
                                Un = SQh()
                                kb.copy(Un[:, :], pp[:, 128:256])
                            else:
                                Un = None
                            yield
                            pq = PS()
                            kb.mm(pq[:, 0:128], Ln[:, :], P[:, :])
                            Pn = SQh()
                            kb.tt(Pn[:, :], pq[:, 0:128], P[:, :], OP.add)
                            P, Lp, Up = Pn, Ln, Un
                            yield
                        BV = SQh()
                        kb.ts(BV[:, :], vtok[:, c, :], bcol, OP.mult, 0.0, OP.add, eng="pool")
                        KE = SQh()
                        kb.ts(KE[:, :], ktok[:, c, :], kes[:, c, h:h + 1], OP.mult, 0.0, OP.add, eng="pool")
                        yield
                        pK = PS()
                        kb.mm(pK[:, 0:128], kT[:, cs], S_g[:, h, :])
                        Rr = SQh()
                        kb.stt(Rr[:, :], pK[:, 0:128], nbeg[:, c, h:h + 1], BV[:, :], OP.mult, OP.add)
                        yield
                        pD = PS()
                        kb.mm(pD[:, 0:128], P[:, :], Rr[:, :])
                        dl = SQh()
                        kb.copy(dl[:, :], pD[:, 0:128], eng="act")
                        yield
                        pS = PS()
                        kb.mm(pS[:, 0:128], KE[:, :], dl[:, :])
                        if main:
                            kb.mm(pS[:, 128:256], AQKT[:, :], dl[:, :])
                            kb.mm(pS[:, 256:384], qT[:, cs], S_g[:, h, :])
                            O2 = SQh()
                            kb.copy(O2[:, :], pS[:, 128:256], eng="act")
                            o = SQh()
                            kb.stt(o[:, :], pS[:, 256:384], eg[:, c, h:h + 1], O2[:, :], OP.mult, OP.add)
                        kb.stt(S_g[:, h, :], S_g[:, h, :], cdt[:, c, h:h + 1], pS[:, 0:128], OP.mult, OP.add)
                        yield
                        if main:
                            c_ = COL()
                            j = SQh()
                            kb.act(j[:, :], o[:, :], AF.Square, accum=c_[:, 0:1])
                            rstd_from_ss(c_[:, 0:1], 128.0, c_[:, 1:2])
                            kb.stt(o[:, :], o[:, :], c_[:, 1:2], gng[:, :], OP.mult, OP.mult)
                            kb.tt(o[:, :], o[:, :], zs[:, c, :], OP.mult)
                            yield
                            pm = PS()
                            kb.tr(pm[:, 0:128], o[:, :], ident)
                            kb.act(mixedT[:, h, cs], pm[:, 0:128], AF.Identity)
                            yield

                for hg in range(2):
                    gens = []
                    for sl in range(4):
                        gdn_pre(hg * 4 + sl, sl)
                        gens.append(gdn_chunks(hg * 4 + sl, sl))
                    while gens:
                        for g_ in list(gens):
                            try:
                                next(g_)
                            except StopIteration:
                                gens.remove(g_)

                CHK('gdn')
                for h in range(4):
                    gam = 1.0 - 2.0 ** (-5.0 - h)
                    qk = []
                    for which in range(2):
                        base = 4112 + which * 1024 + h * 256
                        if which == 0 and not main:
                            qk.append(None)
                            continue
                        raw = []
                        for kc in range(2):
                            wt_ = load_w(w_in, base + kc * 128, 128, WQ())
                            p = PS()
                            proj_fm(wt_, p[:, 0:TB])
                            r_ = FM()
                            kb.act(r_[:, :], p[:, 0:TB], AF.Identity, scale=(1.0 if which == 0 else 1.0 / 16.0))
                            raw.append(r_)
                        a_, b_, c2_, d_ = FM(), FM(), FM(), FM()
                        kb.tt(a_[:, :], raw[0][:, :], cosT[:, :], OP.mult)
                        kb.tt(b_[:, :], raw[1][:, :], sinT[:, :], OP.mult, eng="pool")
                        kb.tt(c2_[:, :], raw[1][:, :], cosT[:, :], OP.mult)
                        kb.tt(d_[:, :], raw[0][:, :], sinT[:, :], OP.mult, eng="pool")
                        kb.tt(a_[:, :], a_[:, :], b_[:, :], OP.subtract)
                        kb.tt(c2_[:, :], c2_[:, :], d_[:, :], OP.add, eng="pool")
                        qk.append((a_, c2_))
                    rq, rk = qk
                    kd = tk[0]
                    for kc in range(2):
                        p = PS()
                        for c in range(NCH):
                            kb.tr(p[:, c * 128:(c + 1) * 128], rk[kc][:, c * 128:(c + 1) * 128], ident)
                        kb.act(kd[:, :, kc * 128:(kc + 1) * 128], r3(p[:, 0:TB], NCH), AF.Identity, scale=RKD(h))
                    vt = tk[1]
                    wv = load_w(w_in, 4112 + 2048 + h * 256, 256, WB1())
                    for tt in range(NCH):
                        p = PS()
                        proj_tm(wv, 256, tt, p[:, 0:256])
                        kb.act(vt[:, tt, :], p[:, 0:256], AF.Identity)
                    if main:
                        gt_ = tk[3]
                        wg = load_w(w_in, 4112 + 3072 + h * 256, 256, WB1())
                        for tt in range(NCH):
                            p = PS()
                            proj_tm(wg, 256, tt, p[:, 0:256])
                            kb.act(gt_[:, tt, :], p[:, 0:256], AF.Silu)
                    for c in range(NCH):
                        cs = slice(c * 128, (c + 1) * 128)
                        if main:
                            pSc = PS()
                            for kc in range(2):
                                kb.mm(pSc[:, 0:128], rk[kc][:, cs], rq[kc][:, cs], start=(kc == 0), stop=(kc == 1))
                            SCT = SQ()
                            kb.tt(SCT[:, :], pSc[:, 0:128], RDT(h), OP.mult)
                            pI = PS()
                            kb.mm(pI[:, 0:256], SCT[:, :], vt[:, c, :])
                            for kc in range(2):
                                kb.mm(pI[:, 256:512], rq[kc][:, cs], S_r[:, h * 2 + kc, :], start=(kc == 0), stop=(kc == 1))
                            Isb = O256()
                            kb.copy(Isb[:, :], pI[:, 0:256], eng="act")
                            o = O256()
                            kb.stt(o[:, :], pI[:, 256:512], RQD(h), Isb[:, :], OP.mult, OP.add)
                        pS = PS()
                        for kc in range(2):
                            kb.mm(pS[:, kc * 256:(kc + 1) * 256], kd[:, c, kc * 128:(kc + 1) * 128], vt[:, c, :])
                        for kc in range(2):
                            kb.stt(S_r[:, h * 2 + kc, :], S_r[:, h * 2 + kc, :], float(gam ** 128),
                                   pS[:, kc * 256:(kc + 1) * 256], OP.mult, OP.add)
                        if main:
                            c_ = COL()
                            j = O256()
                            kb.act(j[:, :], o[:, :], AF.Identity, accum=c_[:, 0:1])
                            kb.act(j[:, :], o[:, :], AF.Square, accum=c_[:, 1:2])
                            kb.ts(c_[:, 2:3], c_[:, 0:1], 1.0 / 256.0, OP.mult)
                            kb.tt(c_[:, 3:4], c_[:, 2:3], c_[:, 2:3], OP.mult)
                            kb.stt(c_[:, 4:5], c_[:, 1:2], 1.0 / 256.0, c_[:, 3:4], OP.mult, OP.subtract)
                            kb.ts(c_[:, 4:5], c_[:, 4:5], EPS, OP.add)
                            kb.act(c_[:, 4:5], c_[:, 4:5], AF.Sqrt)
                            kb.recip(c_[:, 5:6], c_[:, 4:5])
                            kb.ts(o[:, :], o[:, :], c_[:, 2:3], OP.subtract, c_[:, 5:6], OP.mult)
                            kb.tt(o[:, :], o[:, :], rng[:, h * 256:(h + 1) * 256], OP.mult)
                            kb.tt(o[:, :], o[:, :], rnb[:, h * 256:(h + 1) * 256], OP.add, eng="pool")
                            kb.tt(o[:, :], o[:, :], gt_[:, c, :], OP.mult)
                            pm = PS()
                            for kc in range(2):
                                kb.tr(pm[:, kc * 128:(kc + 1) * 128], o[:, kc * 128:(kc + 1) * 128], ident)
                            kb.act(mixedT[:, 8 + h * 2: 10 + h * 2, cs], r3(pm[:, 0:256], 2), AF.Identity)

                CHK('ret')
                if blk == NBLK - 1:
                    kb.ts(S_g[:, :, :], S_g[:, :, :], flag[:, 0:1], OP.mult)
                    kb.ts(S_r[:, :, :], S_r[:, :, :], flag[:, 0:1], OP.mult)
                    kb.ts(carry[:, :, :], carry[:, :, :], flag[:, 0:1], OP.mult)
                if not main:
                    continue
                bi = t0 // TG
                h2T = hT
                for tt in range(NCH):
                    x_ = xt[tt % 2]
                    r0 = t0 + tt * 128
                    kb.dma("sp", x_[:, :], DV(xm[r0: r0 + 128, :]))
                    for dg in range(8):
                        wo = load_w(w_out, dg * 256, 256, WB1())
                        p = PS()
                        for k in range(16):
                            kb.mm(p[:, 0:256], mixedT[:, k, tt * 128:(tt + 1) * 128], wo[:, k, :], start=(k == 0), stop=(k == 15))
                        dsl = slice(dg * 256, (dg + 1) * 256)
                        tmp_ = FM()
                        kb.tt(tmp_[:, :], p[:, 0:256], gt1row[:, dsl], OP.mult)
                        kb.tt(x_[:, dsl], x_[:, dsl], tmp_[:, :], OP.add, eng="pool")
                    kb.dma("sp", View(x2s[bi], x2s_ap[r0: r0 + 128, :]), x_[:, :])
                    norm_mod_T(x_, 32, 48, h2T, tt, f32T=h2f)
                    p = PS()
                    for k in range(16):
                        kb.mm(p[:, 0:32], h2f[:, k, :], wr[:, k, :], start=(k == 0), stop=(k == 15))
                    lg = small
                    kb.tt(lg[:, 0:32], p[:, 0:32], br[:, :], OP.add)
                    c_ = COL()
                    cv_, lv_, lm_ = c_[:, 0:8], lg[:, 0:32], lg[:, 32:64]
                    kb.op("dve", lambda: nc.vector.max(out=cv_.ap, in_=lv_.ap), [lg], [c_])
                    kb.ts(lg[:, 32:64], lg[:, 0:32], c_[:, 3:4], OP.is_ge)
                    kb.ts(lg[:, 0:32], lg[:, 0:32], c_[:, 0:1], OP.subtract)
                    kb.act(lg[:, 0:32], lg[:, 0:32], AF.Exp)
                    kb.tt(lg[:, 0:32], lg[:, 0:32], lg[:, 32:64], OP.mult)
                    c2 = COL()
                    c2v = c2[:, 0:1]
                    kb.op("dve", lambda: nc.vector.tensor_reduce(out=c2v.ap, in_=lv_.ap,
                                                                 axis=mybir.AxisListType.X, op=OP.add), [lg], [c2])
                    kb.recip(c2[:, 1:2], c2[:, 0:1])
                    kb.ts(lg[:, 0:32], lg[:, 0:32], c2[:, 1:2], OP.mult)
                    kb.dma("sp", View(gts[bi], gts_ap[r0: r0 + 128, :]), lg[:, 0:32])
                kb.dma("sp", View(h2s[bi], h2s_ap.rearrange("p (k t) -> p k t", k=16)[:, :, t0:t0 + TB]), h2T[:, :, :])
            kb.barrier()

        with contextlib.ExitStack() as st2:
            fng = kb.sb("fng", [128, D], F32, st2)
            kb.dma("sp", fng[:, :], DV(fng_d[:, :]))
            b1t = kb.sb("b1t", [128, E, 32], F32, st2)
            kb.dma("sp", b1t[:, :, :], DV(b1_d[:, :].rearrange("p (e f) -> p e f", f=32)))
            kb.ts(b1t[:, :, 16:32], b1t[:, :, 16:32], 1.0, OP.add)
            b2t = kb.sb("b2t", [E, D], F32, st2)
            kb.dma("sp", b2t[:, :], DV(b2_d[:, :]))
            h2 = kb.sb("h2", [128, 16, TG], BF16, st2)
            gate = kb.sb("gate", [128, 4, E], F32, st2)
            gateT = kb.sb("gateT", [E, TG], F32, st2)
            acc = [kb.sb("acc%d" % i, [128, D], F32, st2) for i in range(4)]
            actT = [kb.sb("actT%d" % i, [128, 16, TG], BF16, st2) for i in range(1)]
            NEW = 6
            ew = [kb.sb("ew%d" % i, [128, TG], F32, st2) for i in range(NEW)]
            ewi = [0]

            def EW():
                ewi[0] = (ewi[0] + 1) % NEW
                return ew[ewi[0]]
            wb2 = [kb.sb("wb2_%d" % i, [128, 16, 512], BF16, st2) for i in range(3)]
            wb2i = [0]

            def WB2():
                wb2i[0] = (wb2i[0] + 1) % 3
                return wb2[wb2i[0]]
            x2t = [kb.sb("x2t%d" % i, [128, D], F32, st2) for i in range(1)]
            colm = [kb.sb("colm%d" % i, [128, 4], F32, st2) for i in range(4)]
            for tg in range(NTG if do_moe else 0):
                t0 = tg * TG
                kb.dma("sp", h2[:, :, :], View(h2s[tg], h2s_ap.rearrange("p (k t) -> p k t", k=16)[:, :, t0:t0 + TG]))
                kb.dma("sp", gate[:, :, :], View(gts[tg], gts_ap[t0:t0 + TG, :].rearrange("(c p) e -> p c e", p=128)))
                p = PS()
                for tt in range(4):
                    kb.tr(p[0:E, tt * 128:(tt + 1) * 128], gate[:, tt, :], ident)
                kb.copy(gateT[:, :], p[0:E, :])
                for tt in range(4):
                    for dg in range(4):
                        p = PS()
                        kb.mm(p[:, :], gateT[:, tt * 128:(tt + 1) * 128], b2t[:, dg * 512:(dg + 1) * 512])
                        kb.copy(acc[tt][:, dg * 512:(dg + 1) * 512], p[:, :], eng="act")
                for e in range(n_exp):
                    kb.maybe_roll()
                    aT = actT[0]
                    for g in range(4):
                        wg_ = load_w(w1[e], g * 512, 512, WB2())
                        wl_ = load_w(w1[e], D + g * 512, 512, WB2())
                        for j in range(4):
                            fj = g * 4 + j
                            pg = PS()
                            pl = PS()
                            for k in range(16):
                                kb.mm(pg[:, :], wg_[:, k, j * 128:(j + 1) * 128], h2[:, k, :], start=(k == 0), stop=(k == 15))
                            for k in range(16):
                                kb.mm(pl[:, :], wl_[:, k, j * 128:(j + 1) * 128], h2[:, k, :], start=(k == 0), stop=(k == 15))
                            gl, sg, tl = EW(), EW(), EW()
                            kb.ts(gl[:, :], pg[:, :], b1t[:, e, fj:fj + 1], OP.add, 7.0, OP.min)
                            kb.act(sg[:, :], gl[:, :], AF.Sigmoid, scale=1.702)
                            kb.ts(tl[:, :], pl[:, :], b1t[:, e, 16 + fj:17 + fj], OP.add, -6.0, OP.max)
                            kb.tt(sg[:, :], sg[:, :], gl[:, :], OP.mult, eng="pool")
                            kb.stt(aT[:, fj, :], tl[:, :], 8.0, sg[:, :], OP.min, OP.mult)
                    for dg in range(4):
                        w2_ = load_w(w2[e], dg * 512, 512, WB2())
                        for tt in range(4):
                            p = PS()
                            for k in range(16):
                                kb.mm(p[:, :], aT[:, k, tt * 128:(tt + 1) * 128], w2_[:, k, :], start=(k == 0), stop=(k == 15))
                            dsl = slice(dg * 512, (dg + 1) * 512)
                            kb.stt(acc[tt][:, dsl], p[:, :], gate[:, tt, e:e + 1], acc[tt][:, dsl], OP.mult, OP.add)
                for tt in range(4):
                    x_ = x2t[0]
                    r0 = t0 + tt * 128
                    kb.dma("sp", x_[:, :], View(x2s[tg], x2s_ap[r0: r0 + 128, :]))
                    kb.tt(acc[tt][:, :], acc[tt][:, :], gt2row[:, :], OP.mult)
                    kb.tt(x_[:, :], x_[:, :], acc[tt][:, :], OP.add, eng="pool")
                    c_ = colm[tt]
                    kb.act(acc[tt][:, :], x_[:, :], AF.Square, accum=c_[:, 0:1])
                    kb.ts(c_[:, 1:2], c_[:, 0:1], 1.0 / D, OP.mult, EPS, OP.add)
                    kb.act(c_[:, 1:2], c_[:, 1:2], AF.Sqrt)
                    kb.recip(c_[:, 1:2], c_[:, 1:2])
                    kb.stt(x_[:, :], x_[:, :], c_[:, 1:2], fng[:, :], OP.mult, OP.mult)
                    kb.dma("sp", DV(out_d[r0: r0 + 128, :]), x_[:, :])
            kb.barrier()
      except _Stop:
        kb.barrier()
    return nc


h2f_keep = [None]


def make_consts():
    c = np.zeros((128, 1536), np.float32)
    i = np.arange(128)
    c[:, 0:128] = np.eye(128)
    c[:, 128:256] = (i[:, None] <= i[None, :])
    c[:, 256:384] = (i[None, :] < i[:, None])
    c[:, 384:512] = (i[None, :] <= i[:, None])
    c[:, 512:640] = 1.0
    for h in range(4):
        lg = np.log1p(-np.exp2(-5.0 - h))
        d = (i[None, :] - i[:, None]).astype(np.float64)
        m = np.where(d >= 0, np.exp(d * lg), 0.0)
        c[:, 640 + h * 128: 640 + (h + 1) * 128] = m
        c[:, 1152 + h] = np.exp((i + 1.0) * lg)
        c[:, 1156 + h] = np.exp((127.0 - i) * lg)
    c[:, 1160] = np.power(np.float32(10000.0), -np.linspace(0.0, 1.0, 128, dtype=np.float32)).astype(np.float32)
    return c


_NC = [None]


def kernel(x, c, positions, ada_w, ada_b, norm1_g, w_in, conv_w, gdn_a_log, gdn_dt_bias, gdn_norm_g,
           ret_norm_g, ret_norm_b, w_out, norm2_g, w_router, b_router, w1, b1, w2, b2, final_norm_g):
    f = np.float32
    x = np.asarray(x, f)

    def rep(v, n=128):
        v = np.asarray(v, f).reshape(1, -1)
        return np.ascontiguousarray(np.broadcast_to(v, (n, v.shape[1])))

    def pk(v):
        return np.ascontiguousarray(np.asarray(v, f).reshape(-1, 128).T)
    shared = {
        "ada_w": np.ascontiguousarray(np.asarray(ada_w, f)[0]),
        "ada_b": rep(ada_b[0]),
        "n1g": rep(norm1_g[0]), "n2g": rep(norm2_g[0]), "fng": rep(final_norm_g),
        "w_in": np.ascontiguousarray(np.asarray(w_in, f)[0]),
        "convw": np.ascontiguousarray(np.asarray(conv_w, f)[0].T.reshape(24, 128, 4).transpose(1, 0, 2).reshape(128, 96)),
        "alog": rep(gdn_a_log[0]), "dtb": rep(gdn_dt_bias[0]), "gng": rep(gdn_norm_g[0]),
        "rng": rep(ret_norm_g[0]), "rnb": rep(ret_norm_b[0]),
        "w_out": np.ascontiguousarray(np.asarray(w_out, f)[0]),
        "wr": np.ascontiguousarray(np.asarray(w_router, f)[0].reshape(16, 128, 32).transpose(1, 0, 2).reshape(128, 512)),
        "br": rep(b_router[0]),
        "w1": np.ascontiguousarray(np.asarray(w1, f)[0]),
        "b1": np.ascontiguousarray(np.asarray(b1, f)[0].reshape(E, 32, 128).transpose(2, 0, 1).reshape(128, E * 32)),
        "w2": np.ascontiguousarray(np.asarray(w2, f)[0]),
        "b2": np.ascontiguousarray(np.asarray(b2, f)[0]),
        "consts": make_consts(),
    }
    pos = np.asarray(positions, np.int32)
    in_maps = []
    for core in range(8):
        b, half = core // 2, core % 2
        m = dict(shared)
        m["xm"] = np.ascontiguousarray(x[b, half * NTOK:(half + 1) * NTOK])
        m["xp"] = np.ascontiguousarray(x[b, 0:NTOK])
        m["posm"] = np.ascontiguousarray(np.broadcast_to(pos[b, half * NTOK:(half + 1) * NTOK][None, :], (128, NTOK)))
        m["posp"] = np.ascontiguousarray(np.broadcast_to(pos[b, 0:NTOK][None, :], (128, NTOK)))
        m["flag"] = np.full((128, 1), float(half), f)
        m["c"] = pk(np.asarray(c, f)[b])
        in_maps.append(m)
    if _NC[0] is None:
        _NC[0] = build_program()
    res = run_bass_kernel_spmd(_NC[0], in_maps, core_ids=list(range(8)))
    out = np.zeros((4, 2 * NTOK, D), f)
    for core in range(8):
        b, half = core // 2, core % 2
        out[b, half * NTOK:(half + 1) * NTOK] = res.results[core]["out"]
    return out
```
